# Optimizing a Trainium2 kernel written in Bass

```python
import math
import jax
import jax.numpy as jnp
from jax import lax
import numpy as np

D_MODEL = 4096
BATCH = 4
SEQ = 2048
DEPTH = 4

GRID_W = 64
CTX_LEN = 256
EPS = 1e-6
NEG_BIG = -1e30
F_MIN = 1e-30
N_MOD = 6
R_MOD = D_MODEL // 16
GROUP_W = D_MODEL // 4

HG_HD = 128
HG_HEADS = GROUP_W // HG_HD
HG_W = HG_HEADS * HG_HD
HG_CHUNK = 16

NA_HD = 128
NA_HEADS = GROUP_W // NA_HD
NA_W = NA_HEADS * NA_HD
WIN_R = 8
WIN_C = 16

SSM_P = 64
SSM_HEADS = GROUP_W // SSM_P
SSM_D = SSM_HEADS * SSM_P
SSM_G = 2
SSM_N = 128
SSM_CONV = 5
SSM_CONV_CH = SSM_D + 2 * SSM_G * SSM_N
SSM_CHUNK = 64

GQA_HD = 128
GQA_HEADS = GROUP_W // GQA_HD
GQA_KV = GQA_HEADS // 4
GQA_W = GQA_HEADS * GQA_HD
GQA_BLOCK = 128
ROPE_BASE = 10000.0

D_MIX = HG_W + NA_W + SSM_D + GQA_W
IN_SIZES = (5 * HG_W, 3 * NA_W, SSM_D + SSM_CONV_CH + 2 * SSM_HEADS, GQA_W + 2 * GQA_KV * GQA_HD)
P_IN = sum(IN_SIZES)

D_FF = 2 * D_MODEL
N_EXPERTS = 8
TOP_K = 2
D_EXP = 3 * D_MODEL // 8
N_DENSE = (DEPTH + 1) // 2
N_MOE = DEPTH // 2

kernel_name = "hybrid_prefix_diffusion_block"


def rms_norm(x, w):
    xf = x.astype(jnp.float32)
    y = xf * lax.rsqrt(jnp.mean(xf * xf, axis=-1, keepdims=True) + EPS)
    return (y * w.astype(jnp.float32)).astype(x.dtype)


def split_cols(a, sizes):
    return jnp.split(a, np.cumsum(sizes)[:-1].tolist(), axis=-1)


def flip_if(a, rev):
    return jnp.flip(a, axis=1) if rev else a


def adaln_mod(cvec, w_down, w_up, b):
    m = (jax.nn.silu(cvec) @ w_down) @ w_up + b
    return m.reshape(m.shape[:-1] + (N_MOD, D_MODEL))


def modulate(x, w, shift, scale):
    return rms_norm(x, w) * (1 + scale) + shift


def axial_rope(n_tok, head_dim):
    t = jnp.arange(n_tok)
    n_freq = head_dim // 4
    inv = ROPE_BASE ** (-jnp.arange(n_freq, dtype=jnp.float32) / n_freq)
    row = (t // GRID_W).astype(jnp.float32)
    col = (t % GRID_W).astype(jnp.float32)
    ang = jnp.concatenate([row[:, None] * inv, col[:, None] * inv], axis=-1)
    return jnp.cos(ang)[None, :, None, :], jnp.sin(ang)[None, :, None, :]


def apply_rope(x, cos, sin):
    half = x.shape[-1] // 2
    x1, x2 = x[..., :half], x[..., half:]
    return jnp.concatenate([x1 * cos - x2 * sin, x1 * sin + x2 * cos], axis=-1).astype(x.dtype)


def masked_decay(cum, lower):
    diff = cum[:, :, :, None] - cum[:, :, None, :]
    return jnp.where(lower, jnp.exp(jnp.where(lower, diff, 0.0)), 0.0)


def gla_chunked(q, k, v, log_f, s0):
    bsz, t, h, _ = q.shape
    dv = v.shape[-1]
    n = t // HG_CHUNK

    def blk(a):
        return a.astype(jnp.float32).reshape(bsz, n, HG_CHUNK, h, a.shape[-1])

    q, k, v = blk(q), blk(k), blk(v)
    b = jnp.cumsum(blk(log_f), axis=2)
    b_last = b[:, :, -1]
    lower = jnp.tril(jnp.ones((HG_CHUNK, HG_CHUNK), bool))[:, :, None, None]
    decay = masked_decay(b, lower)
    scores = jnp.einsum('bnihk,bnjhk,bnijhk->bnhij', q, k, decay)
    o_intra = jnp.einsum('bnhij,bnjhv->bnihv', scores, v)
    u = jnp.einsum('bnjhk,bnjhv->nbhkv', k * jnp.exp(b_last[:, :, None] - b), v)
    a = jnp.exp(b_last).transpose(1, 0, 2, 3)

    def step(s, inp):
        a_n, u_n = inp
        return a_n[..., None] * s + u_n, s

    s_fin, s_in = lax.scan(step, s0, (a, u))
    o_inter = jnp.einsum('bnihk,nbhkv->bnihv', q * jnp.exp(b), s_in)
    return (o_intra + o_inter).reshape(bsz, t, h, dv), s_fin


def hgrn2_mixer(p_ctx, p_lat, lb, norm_w, with_ctx):
    def heads(a):
        return a.reshape(a.shape[0], a.shape[1], HG_HEADS, HG_HD)

    def prep(p):
        q, f_fw, f_bw, i, g = split_cols(p, (HG_W,) * 5)
        ks, logfs = [], []
        for zf in (f_fw, f_bw):
            zf = zf.astype(jnp.float32)
            f = lb + (1.0 - lb) * jax.nn.sigmoid(zf)
            logfs.append(heads(jnp.log(jnp.maximum(f, F_MIN))))
            ks.append(heads((1.0 - lb) * jax.nn.sigmoid(-zf)))
        return heads(jax.nn.silu(q)), heads(i), g, ks, logfs

    def readout(o, g):
        o = rms_norm(o, norm_w.reshape(HG_HEADS, HG_HD))
        return (o.reshape(g.shape) * jax.nn.silu(g.astype(jnp.float32))).astype(g.dtype)

    qc, vc, gc, kc, lfc = prep(p_ctx)
    ql, vl, gl, kl, lfl = prep(p_lat)
    s0 = jnp.zeros((p_lat.shape[0], HG_HEADS, HG_HD, HG_HD), jnp.float32)
    oc, ol = 0.0, 0.0
    for d in range(2):
        rev = d == 1
        yc, sc = gla_chunked(flip_if(qc, rev), flip_if(kc[d], rev), flip_if(vc, rev), flip_if(lfc[d], rev), s0)
        yl, _ = gla_chunked(flip_if(ql, rev), flip_if(kl[d], rev), flip_if(vl, rev), flip_if(lfl[d], rev), sc)
        ol = ol + flip_if(yl, rev)
        if with_ctx:
            oc = oc + flip_if(yc, rev)
    out_c = readout(oc, gc) if with_ctx else None
    return out_c, readout(ol, gl)


def full_attn(q, k, v):
    s = jnp.einsum('bqhd,bkhd->bhqk', q, k).astype(jnp.float32) * (q.shape[-1] ** -0.5)
    p = jax.nn.softmax(s, axis=-1).astype(v.dtype)
    return jnp.einsum('bhqk,bkhd->bqhd', p, v)


def na_mixer(p_ctx, p_lat, rpb, with_ctx):
    bsz, t, _ = p_lat.shape
    rows = t // GRID_W
    kr = min(WIN_R, rows)
    n_ctx = p_ctx.shape[1]
    scale = NA_HD ** -0.5
    qc, kc, vc = [a.reshape(bsz, n_ctx, NA_HEADS, NA_HD) for a in split_cols(p_ctx, (NA_W,) * 3)]
    ql, kl, vl = [a.reshape(bsz, rows, GRID_W, NA_HEADS, NA_HD) for a in split_cols(p_lat, (NA_W,) * 3)]
    r = jnp.arange(rows)
    row_idx = jnp.clip(r - kr // 2, 0, rows - kr)[:, None] + jnp.arange(kr)[None, :]
    c = jnp.arange(GRID_W)
    col_start = jnp.clip(c - WIN_C // 2, 0, GRID_W - WIN_C)
    col_in = (c[None, :] >= col_start[:, None]) & (c[None, :] < col_start[:, None] + WIN_C)
    k_rows = kl[:, row_idx]
    v_rows = vl[:, row_idx]
    s_lat = jnp.einsum('brchd,brkwhd->bhrckw', ql, k_rows).astype(jnp.float32) * scale
    d_row = row_idx - r[:, None] + (WIN_R - 1)
    d_col = jnp.clip(c[None, :] - c[:, None] + (WIN_C - 1), 0, 2 * WIN_C - 2)
    bias = rpb.astype(jnp.float32)[:, d_row[:, None, :, None], d_col[None, :, None, :]]
    s_lat = jnp.where(col_in[None, None, None, :, None, :], s_lat + bias[None], NEG_BIG)
    s_ctx = jnp.einsum('brchd,blhd->bhrcl', ql, kc).astype(jnp.float32) * scale
    n_win = kr * GRID_W
    s = jnp.concatenate([s_lat.reshape(bsz, NA_HEADS, rows, GRID_W, n_win), s_ctx], axis=-1)
    p = jax.nn.softmax(s, axis=-1).astype(vl.dtype)
    p_lat = p[..., :n_win].reshape(bsz, NA_HEADS, rows, GRID_W, kr, GRID_W)
    o = jnp.einsum('bhrckw,brkwhd->brchd', p_lat, v_rows) + jnp.einsum('bhrcl,blhd->brchd', p[..., n_win:], vc)
    out_l = o.reshape(bsz, t, NA_W)
    out_c = full_attn(qc, kc, vc).reshape(bsz, n_ctx, NA_W) if with_ctx else None
    return out_c, out_l


def dwconv(u, w, b):
    pad = (SSM_CONV - 1) // 2
    y = lax.conv_general_dilated(u, w[:, None, :].astype(u.dtype), window_strides=(1,), padding=[(pad, pad)],
                                 dimension_numbers=('NWC', 'WIO', 'NWC'), feature_group_count=u.shape[-1])
    return y + b.astype(u.dtype)


def ssd_chunked(x, dt, a, bm, cm, s0):
    bsz, t, h, p = x.shape
    g, n_st = bm.shape[2], bm.shape[3]
    r = h // g
    nc = t // SSM_CHUNK
    xr = x.astype(jnp.float32).reshape(bsz, nc, SSM_CHUNK, g, r, p)
    dtr = dt.reshape(bsz, nc, SSM_CHUNK, g, r)
    br = bm.astype(jnp.float32).reshape(bsz, nc, SSM_CHUNK, g, n_st)
    cr = cm.astype(jnp.float32).reshape(bsz, nc, SSM_CHUNK, g, n_st)
    cs = jnp.cumsum(dtr * a.reshape(g, r), axis=2)
    cs_last = cs[:, :, -1]
    lower = jnp.tril(jnp.ones((SSM_CHUNK, SSM_CHUNK), bool))[:, :, None, None]
    decay = masked_decay(cs, lower)
    cb = jnp.einsum('bnigs,bnjgs->bnijg', cr, br)
    xdt = xr * dtr[..., None]
    y_diag = jnp.einsum('bnijgr,bnjgrp->bnigrp', cb[..., None] * decay, xdt)
    u = jnp.einsum('bnjgs,bnjgr,bnjgrp->nbgrps', br, jnp.exp(cs_last[:, :, None] - cs), xdt)
    a_chunk = jnp.exp(cs_last).transpose(1, 0, 2, 3)

    def step(s, inp):
        a_n, u_n = inp
        return a_n[..., None, None] * s + u_n, s

    s_fin, s_in = lax.scan(step, s0, (a_chunk, u))
    y_off = jnp.einsum('bnigs,nbgrps,bnigr->bnigrp', cr, s_in, jnp.exp(cs))
    return (y_diag + y_off).reshape(bsz, t, h, p), s_fin


def ssd_mixer(p_ctx, p_lat, conv_w, conv_b, dt_bias, a_log, d_skip, norm_w, with_ctx):
    def prep(p):
        bsz, t = p.shape[0], p.shape[1]
        z, xbc, dt_raw = split_cols(p, (SSM_D, SSM_CONV_CH, 2 * SSM_HEADS))
        xbc = jax.nn.silu(dwconv(xbc, conv_w, conv_b))
        xs, bm, cm = split_cols(xbc, (SSM_D, SSM_G * SSM_N, SSM_G * SSM_N))
        dt = jax.nn.softplus(dt_raw.astype(jnp.float32).reshape(bsz, t, 2, SSM_HEADS) + dt_bias.astype(jnp.float32))
        return (z, xs.reshape(bsz, t, SSM_HEADS, SSM_P), bm.reshape(bsz, t, SSM_G, SSM_N),
                cm.reshape(bsz, t, SSM_G, SSM_N), dt)

    def readout(y, xs, z):
        y = y + d_skip.astype(jnp.float32)[:, None] * xs.astype(jnp.float32)
        y = y.reshape(z.shape) * jax.nn.silu(z.astype(jnp.float32))
        y = rms_norm(y.reshape(y.shape[:-1] + (SSM_G, SSM_D // SSM_G)), norm_w.reshape(SSM_G, SSM_D // SSM_G))
        return y.reshape(z.shape).astype(z.dtype)

    zc, xc, bc, cc, dtc = prep(p_ctx)
    zl, xl, bl, cl, dtl = prep(p_lat)
    s0 = jnp.zeros((p_lat.shape[0], SSM_G, SSM_HEADS // SSM_G, SSM_P, SSM_N), jnp.float32)
    yc, yl = 0.0, 0.0
    for d in range(2):
        rev = d == 1
        a = -jnp.exp(a_log[d].astype(jnp.float32))
        oc, sc = ssd_chunked(flip_if(xc, rev), flip_if(dtc[:, :, d], rev), a, flip_if(bc, rev), flip_if(cc, rev), s0)
        ol, _ = ssd_chunked(flip_if(xl, rev), flip_if(dtl[:, :, d], rev), a, flip_if(bl, rev), flip_if(cl, rev), sc)
        yl = yl + flip_if(ol, rev)
        if with_ctx:
            yc = yc + flip_if(oc, rev)
    out_c = readout(yc, xc, zc) if with_ctx else None
    return out_c, readout(yl, xl, zl)


def gqa_attend(q, k, v):
    s = jnp.einsum('bqhgd,bkhd->bhgqk', q, k).astype(jnp.float32) * (GQA_HD ** -0.5)
    p = jax.nn.softmax(s, axis=-1).astype(v.dtype)
    return jnp.einsum('bhgqk,bkhd->bqhgd', p, v)


def gqa_mixer(p_ctx, p_lat, qn_w, kn_w, with_ctx):
    def prep(p):
        bsz, t = p.shape[0], p.shape[1]
        q, k, v = split_cols(p, (GQA_W, GQA_KV * GQA_HD, GQA_KV * GQA_HD))
        q = rms_norm(q.reshape(bsz, t, GQA_HEADS, GQA_HD), qn_w)
        k = rms_norm(k.reshape(bsz, t, GQA_KV, GQA_HD), kn_w)
        return q, k, v.reshape(bsz, t, GQA_KV, GQA_HD)

    grp = GQA_HEADS // GQA_KV
    qc, kc, vc = prep(p_ctx)
    ql, kl, vl = prep(p_lat)
    bsz, t = p_lat.shape[0], p_lat.shape[1]
    n_ctx = p_ctx.shape[1]
    cos, sin = axial_rope(t, GQA_HD)
    ql, kl = apply_rope(ql, cos, sin), apply_rope(kl, cos, sin)
    k_all = jnp.concatenate([kc, kl], axis=1)
    v_all = jnp.concatenate([vc, vl], axis=1)
    nb = t // GQA_BLOCK
    q_blocks = ql.reshape(bsz, nb, GQA_BLOCK, GQA_KV, grp, GQA_HD).transpose(1, 0, 2, 3, 4, 5)
    o = lax.map(lambda qb: gqa_attend(qb, k_all, v_all), q_blocks)
    out_l = o.transpose(1, 0, 2, 3, 4, 5).reshape(bsz, t, GQA_W)
    out_c = None
    if with_ctx:
        out_c = gqa_attend(qc.reshape(bsz, n_ctx, GQA_KV, grp, GQA_HD), kc, vc).reshape(bsz, n_ctx, GQA_W)
    return out_c, out_l


def swiglu(h, w1, w3, w2):
    return (jax.nn.silu(h @ w1) * (h @ w3)) @ w2


def moe_swiglu(h, w_router, w1, w3, w2):
    probs = jax.nn.softmax((h @ w_router).astype(jnp.float32), axis=-1)
    top_p, top_i = lax.top_k(probs, TOP_K)
    top_p = top_p / jnp.sum(top_p, axis=-1, keepdims=True)
    gates = jnp.sum(jax.nn.one_hot(top_i, N_EXPERTS, dtype=jnp.float32) * top_p[..., None], axis=-2)
    out = jnp.zeros_like(h)
    for e in range(N_EXPERTS):
        out = out + gates[..., e:e + 1].astype(h.dtype) * swiglu(h, w1[e], w3[e], w2[e])
    return out


def setup_inputs(seed: int = 0) -> dict:
    key = jax.random.key(seed)
    kit = iter(jax.random.split(key, 40))
    f32 = jnp.float32

    def nrm(shape, scale):
        return jax.random.normal(next(kit), shape, f32) * scale

    def gain(shape):
        return 1.0 + nrm(shape, 0.02)

    dt = jnp.exp(jax.random.uniform(next(kit), (DEPTH, 2, SSM_HEADS), f32, math.log(1e-3), math.log(1e-1)))
    return {
        "x": nrm((BATCH, SEQ, D_MODEL), 1.0),
        "c": nrm((BATCH, D_MODEL), 1.0),
        "ctx": nrm((BATCH, CTX_LEN, D_MODEL), 1.0),
        "c_ctx": nrm((D_MODEL,), 1.0),
        "mod_down": nrm((DEPTH, D_MODEL, R_MOD), D_MODEL ** -0.5),
        "mod_up": nrm((DEPTH, R_MOD, N_MOD * D_MODEL), 0.5 * R_MOD ** -0.5),
        "mod_b": nrm((DEPTH, N_MOD * D_MODEL), 0.02),
        "norm1_w": gain((DEPTH, D_MODEL)),
        "norm2_w": gain((DEPTH, D_MODEL)),
        "w_in": nrm((DEPTH, D_MODEL, P_IN), D_MODEL ** -0.5),
        "w_out": nrm((DEPTH, D_MIX, D_MODEL), D_MIX ** -0.5),
        "hgrn_lb_logits": nrm((DEPTH, HG_W), 0.1),
        "hgrn_norm_w": gain((DEPTH, HG_W)),
        "na_rpb": nrm((DEPTH, NA_HEADS, 2 * WIN_R - 1, 2 * WIN_C - 1), 0.1),
        "ssm_conv_w": nrm((DEPTH, SSM_CONV, SSM_CONV_CH), SSM_CONV ** -0.5),
        "ssm_conv_b": nrm((DEPTH, SSM_CONV_CH), 0.02),
        "ssm_dt_bias": dt + jnp.log(-jnp.expm1(-dt)),
        "ssm_a_log": jnp.log(jax.random.uniform(next(kit), (DEPTH, 2, SSM_HEADS), f32, 1.0, 16.0)),
        "ssm_d": gain((DEPTH, SSM_HEADS)),
        "ssm_norm_w": gain((DEPTH, SSM_D)),
        "gqa_q_norm_w": gain((DEPTH, GQA_HD)),
        "gqa_k_norm_w": gain((DEPTH, GQA_HD)),
        "ffn_w1": nrm((N_DENSE, D_MODEL, D_FF), D_MODEL ** -0.5),
        "ffn_w3": nrm((N_DENSE, D_MODEL, D_FF), D_MODEL ** -0.5),
        "ffn_w2": nrm((N_DENSE, D_FF, D_MODEL), D_FF ** -0.5),
        "moe_router": nrm((N_MOE, D_MODEL, N_EXPERTS), D_MODEL ** -0.5),
        "moe_w1": nrm((N_MOE, N_EXPERTS, D_MODEL, D_EXP), D_MODEL ** -0.5),
        "moe_w3": nrm((N_MOE, N_EXPERTS, D_MODEL, D_EXP), D_MODEL ** -0.5),
        "moe_w2": nrm((N_MOE, N_EXPERTS, D_EXP, D_MODEL), D_EXP ** -0.5),
        "final_norm_w": gain((D_MODEL,)),
    }


def reference(x, c, ctx, c_ctx, mod_down, mod_up, mod_b, norm1_w, norm2_w, w_in, w_out,
              hgrn_lb_logits, hgrn_norm_w, na_rpb, ssm_conv_w, ssm_conv_b, ssm_dt_bias, ssm_a_log,
              ssm_d, ssm_norm_w, gqa_q_norm_w, gqa_k_norm_w, ffn_w1, ffn_w3, ffn_w2,
              moe_router, moe_w1, moe_w3, moe_w2, final_norm_w):
    sm = jax.nn.softmax(hgrn_lb_logits.astype(jnp.float32), axis=0)
    lower_bounds = jnp.cumsum(sm, axis=0) - sm[0:1]
    xl, xc = x, ctx
    for l in range(DEPTH):
        with_ctx = l < DEPTH - 1
        ml = adaln_mod(c, mod_down[l], mod_up[l], mod_b[l])[:, :, None, :]
        mc = adaln_mod(c_ctx, mod_down[l], mod_up[l], mod_b[l])
        pl = modulate(xl, norm1_w[l], ml[:, 0], ml[:, 1]) @ w_in[l]
        pc = modulate(xc, norm1_w[l], mc[0], mc[1]) @ w_in[l]
        pl_a, pl_b, pl_c, pl_d = split_cols(pl, IN_SIZES)
        pc_a, pc_b, pc_c, pc_d = split_cols(pc, IN_SIZES)
        outs = [
            hgrn2_mixer(pc_a, pl_a, lower_bounds[l], hgrn_norm_w[l], with_ctx),
            na_mixer(pc_b, pl_b, na_rpb[l], with_ctx),
            ssd_mixer(pc_c, pl_c, ssm_conv_w[l], ssm_conv_b[l], ssm_dt_bias[l], ssm_a_log[l], ssm_d[l],
                      ssm_norm_w[l], with_ctx),
            gqa_mixer(pc_d, pl_d, gqa_q_norm_w[l], gqa_k_norm_w[l], with_ctx),
        ]
        xl = xl + ml[:, 2] * (jnp.concatenate([o[1] for o in outs], axis=-1) @ w_out[l])
        if with_ctx:
            xc = xc + mc[2] * (jnp.concatenate([o[0] for o in outs], axis=-1) @ w_out[l])
        if l % 2 == 0:
            i = l // 2
            ffn = lambda h, i=i: swiglu(h, ffn_w1[i], ffn_w3[i], ffn_w2[i])
        else:
            i = l // 2
            ffn = lambda h, i=i: moe_swiglu(h, moe_router[i], moe_w1[i], moe_w3[i], moe_w2[i])
        xl = xl + ml[:, 5] * ffn(modulate(xl, norm2_w[l], ml[:, 3], ml[:, 4]))
        if with_ctx:
            xc = xc + mc[5] * ffn(modulate(xc, norm2_w[l], mc[3], mc[4]))
    return rms_norm(xl, final_norm_w)
```

```python
import numpy as np
from contextlib import ExitStack
import concourse.bass as bass
import concourse.mybir as mybir
from concourse.bass_utils import run_bass_kernel_spmd


F32 = mybir.dt.float32
BF16 = mybir.dt.bfloat16
AF = mybir.ActivationFunctionType
ALU = mybir.AluOpType
AX = mybir.AxisListType


class Buf:
    __slots__ = ("name", "lw", "rd", "excl")

    def __init__(self, name="", excl=False):
        self.name = name
        self.excl = excl
        self.lw = None
        self.rd = []


class Sched:
    ENG = ("pe", "act", "dve", "pool", "sp")
    N_DMA_SLOTS = 24

    def __init__(self, nc, stack):
        self.nc = nc
        self.stack = stack
        self.prog = {e: [] for e in self.ENG}
        self.sems = {}
        self.cnt = {}
        self.seen = {e: {} for e in self.ENG}
        self.semobj = {}
        self.nsem = 0
        for e in self.ENG:
            self._new_engine_sem(e)
        self.slots = []
        for i in range(self.N_DMA_SLOTS):
            key = self._alloc_sem("dma%d" % i)
            self.slots.append([key, 0])
        self.slot_rr = 0
        self.sw_slots = []
        for i in range(12):
            key = self._alloc_sem("swdma%d" % i)
            self.sw_slots.append([key, 0])
        self.sw_rr = 0

    def _alloc_sem(self, name):
        key = "s%d_%s" % (self.nsem, name)
        self.nsem += 1
        self.semobj[key] = self.stack.enter_context(self.nc.semaphore(key))
        return key

    def _new_engine_sem(self, e):
        self.sems[e] = self._alloc_sem(e)
        self.cnt[e] = 0

    def _waits_for(self, eng, reads, writes):
        need = {}
        def add(tok):
            if tok is None:
                return
            k, v = tok
            if need.get(k, 0) < v:
                need[k] = v
        own = self.sems.get(eng)
        for b in reads:
            add(b.lw)
            if b.excl:
                for t in b.rd:
                    if t[0] != own:
                        add(t)
        strict = getattr(self, "strict", True)
        for b in writes:
            if b.lw is not None and (strict or b.lw[0] != own):
                add(b.lw)
            for t in b.rd:
                if strict or t[0] != own:
                    add(t)
        out = []
        seen = self.seen[eng]
        for k, v in need.items():
            if eng == "pe" and k == self.sems["pe"]:
                continue
            if seen.get(k, 0) >= v:
                continue
            seen[k] = v
            out.append((k, v))
        return out

    def op(self, eng, name, *args, reads=(), writes=(), signal=True, **kw):
        waits = self._waits_for(eng, reads, writes)
        semkey = self.sems[eng]
        if signal:
            self.cnt[eng] += 1
            tok = (semkey, self.cnt[eng])
        else:
            tok = (semkey, self.cnt[eng] + 1)
        semobj = self.semobj
        def run(E):
            for k, v in waits:
                E.wait_ge(semobj[k], v)
            ins = getattr(E, name)(*args, **kw)
            if signal:
                ins.then_inc(semobj[semkey], 1)
        self.prog[eng].append(run)
        self.nins = getattr(self, "nins", 0) + 1
        for b in reads:
            b.rd.append(tok)
        for b in writes:
            b.lw = tok
            b.rd = []
        return tok

    def dma(self, eng, out_ap, in_ap, reads=(), writes=(), **kw):
        if eng == "pool":
            slot = self.sw_slots[self.sw_rr]
            self.sw_rr = (self.sw_rr + 1) % len(self.sw_slots)
        else:
            slot = self.slots[self.slot_rr]
            self.slot_rr = (self.slot_rr + 1) % len(self.slots)
        waits = self._waits_for(eng, reads, writes)
        k, c = slot
        if c > 0 and self.seen[eng].get(k, 0) < c:
            self.seen[eng][k] = c
            waits.append((k, c))
        slot[1] = c + 16
        tok = (k, c + 16)
        semobj = self.semobj
        def run(E, waits=waits, out_ap=out_ap, in_ap=in_ap, k=k, kw=kw):
            for kk, v in waits:
                E.wait_ge(semobj[kk], v)
            E.dma_start(out=out_ap, in_=in_ap, **kw).then_inc(semobj[k], 16)
        self.prog[eng].append(run)
        for b in reads:
            b.rd.append(tok)
        for b in writes:
            b.lw = tok
            b.rd = []
        return tok

    def wait_all(self, eng, toks):
        waits = []
        for tok in toks:
            if tok is None:
                continue
            k, v = tok
            if self.seen[eng].get(k, 0) < v:
                self.seen[eng][k] = v
                waits.append((k, v))
        semobj = self.semobj
        if waits:
            def run(E, waits=waits):
                for k, v in waits:
                    E.wait_ge(semobj[k], v)
            self.prog[eng].append(run)

    def barrier(self):
        toks = [(self.sems[e], self.cnt[e]) for e in self.ENG if self.cnt[e] > 0]
        toks += [(k, c) for k, c in self.slots + self.sw_slots if c > 0]
        for e in self.ENG:
            self.wait_all(e, toks)
        for e in self.ENG:
            if self.cnt[e] > 20000:
                self._new_engine_sem(e)
        for grp, nm in ((self.slots, "dma"), (self.sw_slots, "swdma")):
            for i, sl in enumerate(grp):
                if sl[1] > 20000:
                    sl[0] = self._alloc_sem("%s%d" % (nm, i))
                    sl[1] = 0

    def emit(self):
        nc = self.nc
        with nc.Block() as block:
            @block.tensor
            def _(E):
                for f in self.prog["pe"]:
                    f(E)
            @block.scalar
            def _(E):
                for f in self.prog["act"]:
                    f(E)
            @block.vector
            def _(E):
                for f in self.prog["dve"]:
                    f(E)
            @block.gpsimd
            def _(E):
                for f in self.prog["pool"]:
                    f(E)
            @block.sync
            def _(E):
                for f in self.prog["sp"]:
                    f(E)


class Arena:
    def __init__(self, nc, stack, words):
        self.t = stack.enter_context(nc.sbuf_tensor("arena", [128, words], F32))
        self.words = words
        self.off = 0
        self.marks = []
        self.live = []
        self.freed = []
        self.peak = 0

    def push(self):
        self.marks.append(self.off)

    def pop(self):
        self.off = self.marks.pop()
        keep = []
        for (s, e, b) in self.live:
            if s >= self.off:
                self.freed.append((s, e, b))
            else:
                keep.append((s, e, b))
        self.live = keep

    def alloc(self, shape, dtype, name=""):
        free = int(np.prod(shape[1:]))
        if dtype == BF16:
            words = (free + 1) // 2
        else:
            words = free
        if self.off + words > self.words:
            raise RuntimeError("arena OOM %s need %d at %d/%d" % (name, words, self.off, self.words))
        s0, e0 = self.off, self.off + words
        ap = self.t[0:shape[0], s0:e0]
        self.off += words
        self.peak = max(self.peak, self.off)
        buf = Buf(name)
        nf = []
        for (s, e, b) in self.freed:
            if s < e0 and e > s0:
                if b.lw is not None:
                    buf.rd.append(b.lw)
                buf.rd.extend(b.rd)
                if s >= s0 and e <= e0:
                    continue
            nf.append((s, e, b))
        self.freed = nf
        self.live.append((s0, e0, buf))
        if dtype == BF16:
            ap = ap.bitcast(BF16)
            if free % 2:
                ap = ap[:, 0:free]
        elif dtype != F32:
            ap = ap.bitcast(dtype)
        if len(shape) == 3:
            ap = ap.rearrange("p (a b) -> p a b", a=shape[1])
        elif len(shape) == 4:
            ap = ap.rearrange("p (a b c) -> p a b c", a=shape[1], b=shape[2])
        return ap, buf


D = 4096
NT = 2304
NTILE = 18
NCTX = 256
NLAT = 2048
DEPTH = 4
P_IN = 12320
EPS = 1e-6
D_FF = 8192
N_EXP = 8
D_EXP = 1536

SEGS = [
    ("AQ", 0, 1024, "F"), ("AFF", 1024, 1024, "F"), ("AFB", 2048, 1024, "F"), ("AI", 3072, 1024, "T"), ("AG", 4096, 1024, "F"),
    ("BQ", 5120, 1024, "F"), ("BK", 6144, 1024, "F"), ("BV", 7168, 1024, "T"),
    ("CZ", 8192, 1024, "F"), ("CX", 9216, 1536, "F"), ("CDT", 10752, 32, "T"),
    ("DQ", 10784, 1024, "F"), ("DK", 11808, 256, "F"), ("DV", 12064, 256, "T"),
]
SEG_DT = {"AI": BF16, "BV": BF16, "DV": BF16, "BQ": BF16, "BK": BF16}
SEG_SCALE = {"BQ": 128.0 ** -0.5}


class Prog:
    def __init__(self, layers=(0, 1, 2, 3), dbg=(), mixers=("A", "B", "C", "D"), final=True, arena_words=49000, L=DEPTH, ND=2, NM=2):
        self.L, self.ND, self.NM = L, ND, NM
        self.layers = tuple(layers)
        self.dbg = set(dbg)
        self.mixers = mixers
        self.final = final
        self.nc = nc = bass.Bass("TRN2", target_bir_lowering=False)
        self.stack = ExitStack()
        self.S = Sched(nc, self.stack)
        self.A = Arena(nc, self.stack, arena_words)
        self.ps = []
        for i in range(8):
            t = self.stack.enter_context(nc.psum_tensor("psb%d" % i, [128, 512], F32))
            self.ps.append((t, Buf("ps%d" % i, excl=True)))
        self.ps_rr = 0
        self.in_names = []
        self.out_names = []
        self.rr = 0

    def inp(self, name, shape, dtype=F32):
        if getattr(self, "only_inputs", None) is not None and name not in self.only_inputs:
            return None
        t = self.nc.dram_tensor(name, list(shape), dtype, kind="ExternalInput")
        self.in_names.append(name)
        return t.ap()

    def scratch(self, name, shape, dtype):
        dbg = name in self.dbg
        if name in getattr(self, "ext_in", ()):
            t = self.nc.dram_tensor(name, list(shape), dtype, kind="ExternalInput")
            self.in_names.append(name)
            return t.ap()
        t = self.nc.dram_tensor(name, list(shape), dtype, kind="ExternalOutput" if dbg else "Internal")
        if dbg:
            self.out_names.append(name)
        return t.ap()

    def psum(self, pool=None):
        if pool is None:
            t, b = self.ps[self.ps_rr]
            self.ps_rr = (self.ps_rr + 1) % 8
        elif pool == "s":
            self.ps_s = (getattr(self, "ps_s", -1) + 1) % 4
            t, b = self.ps[self.ps_s]
        else:
            self.ps_a = (getattr(self, "ps_a", -1) + 1) % 4
            t, b = self.ps[4 + self.ps_a]
        return t, b

    def alt(self):
        self.rr ^= 1
        return "act" if self.rr else "dve"

    def copy(self, eng, out, in_, reads, writes, scale=None):
        S = self.S
        if eng == "act":
            if scale is None:
                S.op("act", "activation", out=out, in_=in_, func=AF.Copy, reads=reads, writes=writes)
            else:
                S.op("act", "activation", out=out, in_=in_, func=AF.Copy, scale=float(scale), reads=reads, writes=writes)
        else:
            if scale is None:
                S.op(eng, "tensor_copy", out=out, in_=in_, reads=reads, writes=writes)
            else:
                S.op(eng, "tensor_scalar", out=out, in0=in_, scalar1=float(scale), scalar2=None, op0=ALU.mult,
                     reads=reads, writes=writes)

    def vec_to_fm(self, src_rows, n, dst, dbuf):
        S, A = self.S, self.A
        A.push()
        tmp, tb = A.alloc([128, 128], F32, "v2f")
        S.dma("sp", tmp[0:n, :], src_rows, writes=[tb])
        ps, pb = self.psum()
        S.op("pe", "transpose", out=ps[:, 0:n], in_=tmp[0:n, :], identity=self.ident[0:n, 0:n],
             reads=[tb, self.ident_b], writes=[pb])
        S.op("dve", "tensor_copy", out=dst, in_=ps[:, 0:n], reads=[pb], writes=[dbuf])
        A.pop()

    def declare(self):
        L, ND, NM = self.L, self.ND, self.NM
        self.xin = self.inp("xin", [NT, D])
        self.cvec = self.inp("cvec", [64, 128])
        self.ident_d = self.inp("ident", [128, 128])
        self.mod_down = self.inp("mod_down", [L, D, 256])
        self.mod_up = self.inp("mod_up", [L, 256, 6 * D])
        self.mod_b = self.inp("mod_b", [L, 6 * D])
        self.norm1_w = self.inp("norm1_w", [L, 32, 128])
        self.norm2_w = self.inp("norm2_w", [L, 32, 128])
        self.w_in = self.inp("w_in", [L, D, P_IN])
        self.w_out = self.inp("w_out", [L, D, D])
        self.ffn_w1 = self.inp("ffn_w1", [ND, D, D_FF])
        self.ffn_w3 = self.inp("ffn_w3", [ND, D, D_FF])
        self.ffn_w2 = self.inp("ffn_w2", [ND, D_FF, D])
        self.moe_router = self.inp("moe_router", [NM, D, N_EXP])
        self.moe_w1 = self.inp("moe_w1", [NM, N_EXP, D, D_EXP])
        self.moe_w3 = self.inp("moe_w3", [NM, N_EXP, D, D_EXP])
        self.moe_w2 = self.inp("moe_w2", [NM, N_EXP * D_EXP, D])
        self.final_w = self.inp("final_norm_w", [D])
        self.declare_mixer_inputs()
        t = self.nc.dram_tensor("out", [NLAT, D], F32, kind="ExternalOutput")
        self.out_names.append("out")
        self.out = t.ap()
        self.X = self.scratch("X", [NT, D], F32)
        self.XNT = self.scratch("XNT", [D, NT], BF16)
        self.MODV = self.scratch("MODV", [2, 6 * D], F32)
        self.MIXT = self.scratch("MIXT", [D, NT], BF16)
        self.HT = self.scratch("HT", [N_EXP * D_EXP, NT], BF16)
        self.GT = self.scratch("GT", [N_EXP, NT], F32)
        self.PS = {}
        for (nm, off, n, mode) in SEGS:
            dt_ = SEG_DT.get(nm, F32)
            shp = [n, NT] if mode == "F" else [NT, n]
            self.PS[nm] = self.scratch("P_" + nm, shp, dt_)
        self.declare_mixer_scratch()

    def declare_mixer_inputs(self):
        pass

    def declare_mixer_scratch(self):
        pass

    def setup_consts(self):
        S, A = self.S, self.A
        self.ident, self.ident_b = A.alloc([128, 128], F32, "ident")
        S.dma("sp", self.ident, self.ident_d, writes=[self.ident_b])
        self.identb, self.identb_b = A.alloc([128, 128], BF16, "identb")
        S.op("dve", "tensor_copy", out=self.identb, in_=self.ident, reads=[self.ident_b], writes=[self.identb_b])
        self.eps_t, self.eps_b = A.alloc([128, 1], F32, "eps")
        S.op("pool", "memset", self.eps_t, EPS, writes=[self.eps_b])
        self.onesb, self.onesb_b = A.alloc([128, 128], BF16, "onesb")
        S.op("pool", "memset", self.onesb, 1.0, writes=[self.onesb_b])
        self.csT, self.csT_b = A.alloc([128, 64], F32, "csT")
        self.vec_to_fm(self.cvec, 64, self.csT, self.csT_b)
        S.op("act", "activation", out=self.csT, in_=self.csT, func=AF.Silu, reads=[self.csT_b], writes=[self.csT_b])

    def stage_init(self):
        for i in range(NTILE):
            self.S.dma("sp", self.X[i * 128:(i + 1) * 128, :], self.xin[i * 128:(i + 1) * 128, :])
        self.S.barrier()

    def stage_adaln(self, l):
        S, A = self.S, self.A
        A.push()
        dn, dnb = A.alloc([128, 32, 256], F32, "mod_down")
        S.dma("sp", dn, self.mod_down[l].rearrange("(kc p) r -> p kc r", p=128), writes=[dnb])
        cs3 = self.csT.rearrange("p (s c) -> p s c", s=2)
        ps, pb = self.psum()
        for kc in range(32):
            S.op("pe", "matmul", ps[0:2, 0:256], lhsT=cs3[:, :, kc], rhs=dn[:, kc, :], start=(kc == 0), stop=(kc == 31),
                 reads=[self.csT_b, dnb], writes=[pb], signal=(kc == 31))
        h, hb = A.alloc([128, 256], F32, "h")
        S.op("dve", "tensor_copy", out=h[0:2, :], in_=ps[0:2, 0:256], reads=[pb], writes=[hb])
        hT, hTb = A.alloc([128, 2, 2], F32, "hT")
        for rc in range(2):
            ps2, pb2 = self.psum()
            S.op("pe", "transpose", out=ps2[:, 0:2], in_=h[0:2, rc * 128:(rc + 1) * 128], identity=self.ident[0:2, 0:2],
                 reads=[hb, self.ident_b], writes=[pb2])
            S.op("dve", "tensor_copy", out=hT[:, rc, :], in_=ps2[:, 0:2], reads=[pb2], writes=[hTb])
        ups = [A.alloc([128, 2, D], F32, "up%d" % i) for i in range(2)]
        bts = [A.alloc([128, D], F32, "modb%d" % i) for i in range(2)]
        mts = [A.alloc([128, D], F32, "mt%d" % i) for i in range(2)]
        for j in range(6):
            up, upb = ups[j % 2]
            bt, btb = bts[j % 2]
            mt, mtb = mts[j % 2]
            S.dma("sp", up, self.mod_up[l, :, j * D:(j + 1) * D].rearrange("(rc p) n -> p rc n", p=128), writes=[upb])
            S.dma("sp", bt[0:2, :], self.mod_b[l, j * D:(j + 1) * D].partition_broadcast(2), writes=[btb])
            for cb in range(8):
                ps3, pb3 = self.psum()
                for rc in range(2):
                    S.op("pe", "matmul", ps3[0:2, :], lhsT=hT[:, rc, :], rhs=up[:, rc, cb * 512:(cb + 1) * 512],
                         start=(rc == 0), stop=(rc == 1), reads=[hTb, upb], writes=[pb3], signal=(rc == 1))
                S.op("dve", "tensor_tensor", out=mt[0:2, cb * 512:(cb + 1) * 512], in0=ps3[0:2, :],
                     in1=bt[0:2, cb * 512:(cb + 1) * 512], op=ALU.add, reads=[pb3, btb], writes=[mtb])
            S.dma("sp", self.MODV[:, j * D:(j + 1) * D], mt[0:2, :], reads=[mtb])
        A.pop()
        S.barrier()

    def load_layer_params(self, l):
        S, A = self.S, self.A
        self.modfm = {}
        for j in range(6):
            for s in range(2):
                t, b = A.alloc([128, 32], F32, "modfm%d%d" % (j, s))
                self.vec_to_fm(self.MODV[s, j * D:(j + 1) * D].rearrange("(c p) -> c p", p=128), 32, t, b)
                self.modfm[(j, s)] = (t, b)
        n1, n1b = A.alloc([128, 32], F32, "n1")
        self.vec_to_fm(self.norm1_w[l], 32, n1, n1b)
        n2, n2b = A.alloc([128, 32], F32, "n2")
        self.vec_to_fm(self.norm2_w[l], 32, n2, n2b)
        self.wm = {}
        for (k, nw, nwb, js) in ((1, n1, n1b, 1), (2, n2, n2b, 4)):
            for s in range(2):
                t, b = A.alloc([128, 32], F32, "wm%d%d" % (k, s))
                sc, scb = self.modfm[(js, s)]
                S.op("dve", "scalar_tensor_tensor", out=t, in0=sc, scalar=1.0, in1=nw, op0=ALU.add, op1=ALU.mult,
                     reads=[scb, nwb], writes=[b])
                self.wm[(k, s)] = (t, b)

    def stage_norm(self, k, moe_idx=None):
        S, A = self.S, self.A
        jshift = 0 if k == 1 else 3
        A.push()
        xts = [A.alloc([128, D], F32, "nx%d" % i) for i in range(2)]
        junk, junkb = A.alloc([128, D], BF16, "junk")
        x32 = [[A.alloc([128, 16, 128], F32, "x32_%d_%d" % (i, e)) for e in range(2)] for i in range(2)]
        xbs = [A.alloc([128, 32, 128], BF16, "xb%d" % i) for i in range(2)]
        sm = [A.alloc([128, 4], F32, "sm%d" % i) for i in range(2)]
        if moe_idx is not None:
            rt, rtb = A.alloc([128, 32, N_EXP], F32, "router")
            S.dma("sp", rt, self.moe_router[moe_idx].rearrange("(kc p) e -> p kc e", p=128), writes=[rtb])
            gT, gTb = A.alloc([128, NT], F32, "gT")
            gw = [A.alloc([128, 64], F32, "gw%d" % i) for i in range(2)]
        xntv = self.XNT.rearrange("(c p) t -> p c t", p=128)
        import os
        for i in range(int(os.environ.get('NORM_TILES', NTILE))):
            s = 1 if i < 2 else 0
            xt, xtb = xts[i % 2]
            st, stb = sm[i % 2]
            wm, wmb = self.wm[(k, s)]
            sh, shb = self.modfm[(jshift, s)]
            S.dma("sp", xt, self.X[i * 128:(i + 1) * 128, :], writes=[xtb])
            S.op("act", "activation", out=junk, in_=xt, func=AF.Square, accum_out=st[:, 0:1],
                 reads=[xtb], writes=[junkb, stb])
            S.op("act", "activation", out=st[:, 1:2], in_=st[:, 0:1], func=AF.Sqrt, bias=self.eps_t, scale=1.0 / D,
                 reads=[stb, self.eps_b], writes=[stb])
            S.op("dve", "reciprocal", out=st[:, 2:3], in_=st[:, 1:2], reads=[stb], writes=[stb])
            S.op("dve", "tensor_scalar", out=xt, in0=xt, scalar1=st[:, 2:3], scalar2=None, op0=ALU.mult,
                 reads=[xtb, stb], writes=[xtb])
            (xa, xab), (xd, xdb) = x32[i % 2]
            CUT = int(os.environ.get('NORM_CUT', 9))
            if CUT < 2:
                continue
            for c4 in range(8):
                ps, pb = self.psum()
                for q in range(4):
                    c = c4 * 4 + q
                    S.op("pe", "transpose", out=ps[:, q * 128:(q + 1) * 128], in_=xt[:, c * 128:(c + 1) * 128],
                         identity=self.ident, reads=[xtb, self.ident_b], writes=[pb], signal=(q == 3))
                for q in range(4):
                    c = c4 * 4 + q
                    j = (c4 // 2) * 4 + q
                    if c4 % 2 == 0:
                        S.op("act", "activation", out=xa[:, j, :], in_=ps[:, q * 128:(q + 1) * 128], func=AF.Identity,
                             scale=wm[:, c:c + 1], bias=sh[:, c:c + 1], reads=[pb, wmb, shb], writes=[xab])
                    else:
                        S.op("dve", "tensor_scalar", out=xd[:, j, :], in0=ps[:, q * 128:(q + 1) * 128],
                             scalar1=wm[:, c:c + 1], scalar2=sh[:, c:c + 1], op0=ALU.mult, op1=ALU.add,
                             reads=[pb, wmb, shb], writes=[xdb])
            if CUT < 3:
                continue
            xb, xbb = xbs[i % 2]
            xb5 = xb.rearrange("p (g e q) t -> p g e q t", g=4, e=2)
            S.op("pool", "tensor_copy", out=xb5[:, :, 0, :, :], in_=xa.rearrange("p (g q) t -> p g q t", g=4), reads=[xab], writes=[xbb])
            S.op("pool", "tensor_copy", out=xb5[:, :, 1, :, :], in_=xd.rearrange("p (g q) t -> p g q t", g=4), reads=[xdb], writes=[xbb])
            if CUT < 4:
                continue
            S.dma("sp", xntv[:, :, i * 128:(i + 1) * 128], xb, reads=[xbb])
            if moe_idx is not None:
                ps, pb = self.psum()
                for c in range(32):
                    src = xa if (c // 4) % 2 == 0 else xd
                    srcb = xab if (c // 4) % 2 == 0 else xdb
                    S.op("pe", "matmul", ps[:, 0:N_EXP], lhsT=src[:, (c // 8) * 4 + c % 4, :], rhs=rt[:, c, :], start=(c == 0), stop=(c == 31),
                         reads=[srcb, rtb], writes=[pb], signal=(c == 31))
                g, gb = gw[i % 2]
                lg = g[:, 0:8]; e_ = g[:, 8:16]; m1 = g[:, 16:17]; nm1 = g[:, 17:18]; mk = g[:, 24:32]
                l2 = g[:, 32:40]; m2 = g[:, 18:19]; sel = g[:, 40:48]; den = g[:, 19:20]; rden = g[:, 20:21]; gt = g[:, 48:56]
                S.op("dve", "tensor_copy", out=lg, in_=ps[:, 0:N_EXP], reads=[pb], writes=[gb])
                S.op("dve", "tensor_reduce", out=m1, in_=lg, axis=AX.X, op=ALU.max, reads=[gb], writes=[gb])
                S.op("dve", "tensor_scalar", out=nm1, in0=m1, scalar1=-1.0, scalar2=None, op0=ALU.mult, reads=[gb], writes=[gb])
                S.op("act", "activation", out=e_, in_=lg, func=AF.Exp, bias=nm1, scale=1.0, reads=[gb], writes=[gb])
                S.op("dve", "tensor_scalar", out=mk, in0=lg, scalar1=m1, scalar2=None, op0=ALU.is_equal, reads=[gb], writes=[gb])
                S.op("dve", "scalar_tensor_tensor", out=l2, in0=mk, scalar=-1e30, in1=lg, op0=ALU.mult, op1=ALU.add,
                     reads=[gb], writes=[gb])
                S.op("dve", "tensor_reduce", out=m2, in_=l2, axis=AX.X, op=ALU.max, reads=[gb], writes=[gb])
                S.op("dve", "tensor_scalar", out=sel, in0=lg, scalar1=m2, scalar2=None, op0=ALU.is_ge, reads=[gb], writes=[gb])
                S.op("dve", "tensor_tensor", out=sel, in0=sel, in1=e_, op=ALU.mult, reads=[gb], writes=[gb])
                S.op("dve", "tensor_reduce", out=den, in_=sel, axis=AX.X, op=ALU.add, reads=[gb], writes=[gb])
                S.op("dve", "reciprocal", out=rden, in_=den, reads=[gb], writes=[gb])
                S.op("dve", "tensor_scalar", out=gt, in0=sel, scalar1=rden, scalar2=None, op0=ALU.mult, reads=[gb], writes=[gb])
                ps2, pb2 = self.psum()
                S.op("pe", "transpose", out=ps2[0:N_EXP, 0:128], in_=gt, identity=self.ident, reads=[gb, self.ident_b], writes=[pb2])
                S.op("dve", "tensor_copy", out=gT[0:N_EXP, i * 128:(i + 1) * 128], in_=ps2[0:N_EXP, 0:128], reads=[pb2], writes=[gTb])
        if moe_idx is not None:
            S.dma("sp", self.GT, gT[0:N_EXP, :], reads=[gTb])
        A.pop()
        S.barrier()

    def gemm(self, xT, K, groups, Tb, WB, T=NT, wq="pool"):
        S, A = self.S, self.A
        KC = K // 128
        A.push()
        nmem = max(len(g["members"]) for g in groups)
        hk = 16 if KC % 16 == 0 else KC
        NWP = KC // hk
        wbufs = [[[A.alloc([128, hk, WB], BF16, "w%d_%d_%d" % (m, i, hh)) for hh in range(NWP)] for i in range(2)]
                 for m in range(nmem)]
        wpar = [0] * nmem
        xv = xT.rearrange("(kc p) t -> p kc t", p=128)
        NXS = 4
        kstep = KC // NXS
        for tb0 in range(0, T, Tb):
            tbn = min(Tb, T - tb0)
            A.push()
            xparts = []
            for xi in range(NXS):
                xt, xtb = A.alloc([128, kstep, tbn], BF16, "xt%d" % xi)
                S.dma("sp", xt, xv[:, xi * kstep:(xi + 1) * kstep, tb0:tb0 + tbn], writes=[xtb])
                xparts.append((xt, xtb))
            for g in groups:
                n = g["n"]
                if g.get("pre"):
                    g["pre"](tb0, tbn)
                for c0 in range(0, n, WB):
                    wn = min(WB, n - c0)
                    if g.get("precol"):
                        g["precol"](c0, wn, tb0)
                    cur = []
                    for m, (W, epi) in enumerate(g["members"]):
                        halves = wbufs[m][wpar[m]]
                        wpar[m] ^= 1
                        wv = W.rearrange("(kc p) n -> p kc n", p=128)
                        for hh in range(NWP):
                            wt, wtb = halves[hh]
                            S.dma(wq, wt[:, :, 0:wn], wv[:, hh * hk:(hh + 1) * hk, c0:c0 + wn], writes=[wtb])
                        cur.append((halves, epi))
                    if g["mode"] == "F":
                        for cc in range(0, wn, 128):
                            cn = min(128, wn - cc)
                            for t0 in range(0, tbn, 512):
                                nt = min(512, tbn - t0)
                                for (halves, epi) in cur:
                                    ps, pb = self.psum()
                                    for kc in range(KC):
                                        wt, wtb = halves[kc // hk]
                                        xt, xtb = xparts[kc // kstep]
                                        S.op("pe", "matmul", ps[0:cn, 0:nt], lhsT=wt[:, kc % hk, cc:cc + cn],
                                             rhs=xt[:, kc % kstep, t0:t0 + nt], start=(kc == 0), stop=(kc == KC - 1),
                                             reads=[wtb, xtb], writes=[pb], signal=(kc == KC - 1))
                                    epi(c0 + cc, cn, tb0 + t0, nt, ps, pb)
                    else:
                        for t0 in range(0, tbn, 128):
                            for (halves, epi) in cur:
                                ps, pb = self.psum()
                                for kc in range(KC):
                                    wt, wtb = halves[kc // hk]
                                    xt, xtb = xparts[kc // kstep]
                                    S.op("pe", "matmul", ps[:, 0:wn], lhsT=xt[:, kc % kstep, t0:t0 + 128],
                                         rhs=wt[:, kc % hk, 0:wn], start=(kc == 0), stop=(kc == KC - 1),
                                         reads=[wtb, xtb], writes=[pb], signal=(kc == KC - 1))
                                epi(c0, wn, tb0 + t0, ps, pb)
            A.pop()
        A.pop()

    def stage_win(self, l):
        S, A = self.S, self.A
        A.push()
        tmpsF = [A.alloc([128, 512], F32, "ef%d" % i) for i in range(3)]
        st = {"i": 0}

        def mk_epi_F(nm):
            dst = self.PS[nm]
            dt_ = SEG_DT.get(nm, F32)
            sc = SEG_SCALE.get(nm)
            def epi(c0, cn, t0, nt, ps, pb):
                tmp, tb = tmpsF[st["i"] % 3]
                st["i"] += 1
                tv = tmp.bitcast(BF16)[:, 0:512] if dt_ == BF16 else tmp
                self.copy(self.alt(), tv[0:cn, 0:nt], ps[0:cn, 0:nt], [pb], [tb], scale=sc)
                S.dma("sp", dst[c0:c0 + cn, t0:t0 + nt], tv[0:cn, 0:nt], reads=[tb])
            return epi

        def mk_epi_T(nm):
            dst = self.PS[nm]
            dt_ = SEG_DT.get(nm, F32)
            def epi(c0, cn, t0, ps, pb):
                tmp, tb = tmpsF[st["i"] % 3]
                st["i"] += 1
                tv = tmp.bitcast(BF16)[:, 0:512] if dt_ == BF16 else tmp
                self.copy(self.alt(), tv[:, 0:cn], ps[:, 0:cn], [pb], [tb])
                S.dma("sp", dst[t0:t0 + 128, c0:c0 + cn], tv[:, 0:cn], reads=[tb])
            return epi

        groups = []
        for (nm, off, n, mode) in SEGS:
            W = self.w_in[l, :, off:off + n]
            groups.append(dict(mode=mode, n=n, members=[(W, mk_epi_F(nm) if mode == "F" else mk_epi_T(nm))]))
        self.gemm(self.XNT, D, groups, Tb=1152, WB=256)
        A.pop()
        S.barrier()

    def resid_group(self, W, n, jgate, WB):
        S, A = self.S, self.A
        gts = [[A.alloc([128, WB], F32, "gbc%d_%d" % (i, s_)) for s_ in range(2)] for i in range(2)]
        xrs = [A.alloc([128, WB], F32, "xr%d" % i) for i in range(3)]
        tms = [A.alloc([128, WB], F32, "tm%d" % i) for i in range(2)]
        st = {"g": 0, "i": 0, "cur": None}

        def precol(c0, wn, tb0):
            pair = gts[st["g"] % 2]
            st["g"] += 1
            for s in range(2):
                gt, gtb = pair[s]
                S.dma("sp", gt[:, 0:wn], self.MODV[s, jgate * D + c0: jgate * D + c0 + wn].partition_broadcast(128),
                      writes=[gtb])
            st["cur"] = pair

        def epi(c0, cn, t0, ps, pb):
            s = 1 if t0 < NCTX else 0
            gt, gtb = st["cur"][s]
            xr, xrb = xrs[st["i"] % 3]
            tm, tmb = tms[st["i"] % 2]
            st["i"] += 1
            S.dma("sp", xr[:, 0:cn], self.X[t0:t0 + 128, c0:c0 + cn], writes=[xrb])
            S.op("dve", "tensor_tensor", out=tm[:, 0:cn], in0=ps[:, 0:cn], in1=gt[:, 0:cn], op=ALU.mult,
                 reads=[pb, gtb], writes=[tmb])
            S.op("dve", "tensor_tensor", out=xr[:, 0:cn], in0=xr[:, 0:cn], in1=tm[:, 0:cn], op=ALU.add,
                 reads=[xrb, tmb], writes=[xrb])
            S.dma("sp", self.X[t0:t0 + 128, c0:c0 + cn], xr[:, 0:cn], reads=[xrb])
        return dict(mode="T", n=n, members=[(W, epi)], precol=precol)

    def stage_wout(self, l):
        self.A.push()
        g = self.resid_group(self.w_out[l], D, 2, 512)
        self.gemm(self.MIXT, D, [g], Tb=1152, WB=512)
        self.A.pop()
        self.S.barrier()

    def stage_ffn1(self, l):
        S, A = self.S, self.A
        moe = (l % 2 == 1)
        i = l // 2
        A.push()
        s1s = [A.alloc([128, 512], F32, "s1_%d" % k) for k in range(2)]
        t1s = [A.alloc([128, 512], F32, "t1_%d" % k) for k in range(2)]
        abs_ = [A.alloc([128, 512], BF16, "ab_%d" % k) for k in range(3)]
        st = {"s": 0, "a": 0, "cur": None, "g": None, "tb0": 0}
        if moe:
            gbs = [A.alloc([128, 1152], F32, "gate%d" % k) for k in range(2)]
            st["gi"] = 0

        def mk(e):
            row0 = e * D_EXP if moe else 0
            def epi1(c0, cn, t0, nt, ps, pb):
                s1, s1b = s1s[st["s"] % 2]
                st["s"] += 1
                S.op("act", "activation", out=s1[0:cn, 0:nt], in_=ps[0:cn, 0:nt], func=AF.Silu, reads=[pb], writes=[s1b])
                st["cur"] = (s1, s1b)
            def epi3(c0, cn, t0, nt, ps, pb):
                s1, s1b = st["cur"]
                ab, abb = abs_[st["a"] % 3]
                if moe:
                    t1, t1b = t1s[st["a"] % 2]
                    gb_, gbb = st["g"]
                    S.op("dve", "tensor_tensor", out=t1[0:cn, 0:nt], in0=s1[0:cn, 0:nt], in1=ps[0:cn, 0:nt], op=ALU.mult,
                         reads=[s1b, pb], writes=[t1b])
                    tl = t0 - st["tb0"]
                    S.op("pool", "tensor_tensor", out=ab[0:cn, 0:nt], in0=t1[0:cn, 0:nt], in1=gb_[0:cn, tl:tl + nt], op=ALU.mult,
                         reads=[t1b, gbb], writes=[abb])
                else:
                    S.op("dve", "tensor_tensor", out=ab[0:cn, 0:nt], in0=s1[0:cn, 0:nt], in1=ps[0:cn, 0:nt], op=ALU.mult,
                         reads=[s1b, pb], writes=[abb])
                st["a"] += 1
                S.dma("sp", self.HT[row0 + c0:row0 + c0 + cn, t0:t0 + nt], ab[0:cn, 0:nt], reads=[abb])
            def pre(tb0, tbn):
                gb_, gbb = gbs[st["gi"] % 2]
                st["gi"] += 1
                S.dma("sp", gb_[:, 0:tbn], self.GT[e, tb0:tb0 + tbn].partition_broadcast(128), writes=[gbb])
                st["g"] = (gb_, gbb)
                st["tb0"] = tb0
            return epi1, epi3, pre

        groups = []
        if moe:
            for e in range(N_EXP):
                e1, e3, pre = mk(e)
                groups.append(dict(mode="F", n=D_EXP, members=[(self.moe_w1[i, e], e1), (self.moe_w3[i, e], e3)], pre=pre))
        else:
            e1, e3, _ = mk(0)
            groups.append(dict(mode="F", n=D_FF, members=[(self.ffn_w1[i], e1), (self.ffn_w3[i], e3)]))
        self.gemm(self.XNT, D, groups, Tb=1152, WB=256)
        A.pop()
        S.barrier()

    def stage_ffn2(self, l):
        moe = (l % 2 == 1)
        i = l // 2
        self.A.push()
        if moe:
            K = N_EXP * D_EXP
            g = self.resid_group(self.moe_w2[i], D, 5, 256)
            self.gemm(self.HT[0:K, :], K, [g], Tb=256, WB=256)
        else:
            K = D_FF
            g = self.resid_group(self.ffn_w2[i], D, 5, 256)
            self.gemm(self.HT[0:K, :], K, [g], Tb=512, WB=256)
        self.A.pop()
        self.S.barrier()

    def stage_final(self):
        S, A = self.S, self.A
        A.push()
        fw, fwb = A.alloc([128, D], F32, "fw")
        S.dma("sp", fw, self.final_w.partition_broadcast(128), writes=[fwb])
        xts = [A.alloc([128, D], F32, "fx%d" % i) for i in range(2)]
        junk, junkb = A.alloc([128, D], BF16, "junk")
        sm = [A.alloc([128, 4], F32, "fsm%d" % i) for i in range(2)]
        for i in range(2, NTILE):
            xt, xtb = xts[i % 2]
            st, stb = sm[i % 2]
            S.dma("sp", xt, self.X[i * 128:(i + 1) * 128, :], writes=[xtb])
            S.op("act", "activation", out=junk, in_=xt, func=AF.Square, accum_out=st[:, 0:1], reads=[xtb], writes=[junkb, stb])
            S.op("act", "activation", out=st[:, 1:2], in_=st[:, 0:1], func=AF.Sqrt, bias=self.eps_t, scale=1.0 / D,
                 reads=[stb, self.eps_b], writes=[stb])
            S.op("dve", "reciprocal", out=st[:, 2:3], in_=st[:, 1:2], reads=[stb], writes=[stb])
            S.op("dve", "scalar_tensor_tensor", out=xt, in0=xt, scalar=st[:, 2:3], in1=fw, op0=ALU.mult, op1=ALU.mult,
                 reads=[xtb, stb, fwb], writes=[xtb])
            S.dma("sp", self.out[(i - 2) * 128:(i - 1) * 128, :], xt, reads=[xtb])
        A.pop()
        S.barrier()

    def stage_mixers(self, l):
        pass

    def build(self, stop_after=None, only_mixers=None):
        self.declare()
        self.setup_consts()
        if only_mixers is not None:
            self.stage_mixers(only_mixers)
            self.S.barrier()
            self.S.emit()
            self.stack.close()
            return self.nc
        self.stage_init()
        def stages():
            for l in self.layers:
                yield "adaln", lambda: self.stage_adaln(l)
                self.A.push()
                yield "params", lambda: self.load_layer_params(l)
                yield "norm1", lambda: self.stage_norm(1)
                yield "win", lambda: self.stage_win(l)
                yield "mixers", lambda: self.stage_mixers(l)
                yield "wout", lambda: self.stage_wout(l)
                yield "norm2", lambda: self.stage_norm(2, moe_idx=(l // 2 if l % 2 == 1 else None))
                yield "ffn1", lambda: self.stage_ffn1(l)
                yield "ffn2", lambda: self.stage_ffn2(l)
                self.A.pop()
            if self.final:
                yield "final", lambda: self.stage_final()
        for nm, f in stages():
            if getattr(self, "only_stages", None) is None or nm in self.only_stages:
                f()
            if stop_after == nm:
                break
        self.S.barrier()
        self.S.emit()
        self.stack.close()
        return self.nc


HD = 128
NEG = -30000.0
NA_DELTAS = (-4, -2, 0, 2, 4, 6, 8, 10)


def na_chunk_tiles(ci):
    qr0 = 8 * ci
    lo = min(max(qr0 - 4, 0), 24)
    hi = min(max(qr0 + 3, 0), 24) + 7
    return list(range((lo // 2) * 2, hi + 1, 2))


def na_mask_const():
    m = np.full((4, 8, 128, 512), NEG, np.float32)
    for ci in range(4):
        qr0 = 8 * ci
        for ti, kr0 in enumerate(na_chunk_tiles(ci)):
            for pp in range(128):
                p = 127 - pp
                a, kc = p // 64, p % 64
                kr = kr0 + a
                for b in range(8):
                    qr = qr0 + b
                    st = min(max(qr - 4, 0), 24)
                    if not (st <= kr <= st + 7):
                        continue
                    qc = np.arange(64)
                    cs = np.clip(qc - 8, 0, 48)
                    ok = (kc >= cs) & (kc < cs + 16)
                    m[ci, ti, pp, b * 64:(b + 1) * 64] = np.where(ok, 0.0, NEG)
    return m


def na_rpb_table(rpb):
    L, H = rpb.shape[:2]
    F = np.zeros((L, H, 23, 127), np.float32)
    F[:, :, 4:19, 48:79] = rpb[:, :, ::-1, ::-1]
    return F


def rope_tables():
    t = np.arange(NLAT)
    nf = HD // 4
    inv = (10000.0 ** (-np.arange(nf, dtype=np.float32) / nf)).astype(np.float32)
    row = (t // 64).astype(np.float32)
    col = (t % 64).astype(np.float32)
    ang = np.concatenate([row[:, None] * inv, col[:, None] * inv], -1)
    cos = np.cos(ang).astype(np.float32).T
    sin = np.sin(ang).astype(np.float32).T
    return np.concatenate([cos, cos], 0), np.concatenate([sin, sin], 0)


def rot_T():
    R = np.zeros((128, 128), np.float32)
    for m in range(64):
        R[m, m + 64] = -1.0
        R[m + 64, m] = 1.0
    return R.T.copy()


def mixer_inputs_np(params):
    L = params["ssm_conv_w"].shape[0]
    cosT, sinT = rope_tables()
    t = np.arange(128)
    m32u = ((t[:, None] // 32 == t[None, :] // 32) & (t[:, None] <= t[None, :])).astype(np.float32)
    m32l = ((t[:, None] // 32 == t[None, :] // 32) & (t[:, None] >= t[None, :])).astype(np.float32)
    sel = np.zeros((32, 32, 128), np.float32)
    for c in range(32):
        sel[c, c, :] = 1.0
    return dict(
        ident=np.eye(128, dtype=np.float32),
        gqa_q_norm_w=np.ascontiguousarray(params["gqa_q_norm_w"]), gqa_k_norm_w=np.ascontiguousarray(params["gqa_k_norm_w"]),
        rope_cos=cosT, rope_sin=sinT, rotT=rot_T(), exch=np.eye(128, dtype=np.float32)[::-1].copy(),
        na_mask=na_mask_const(), na_rpbF=na_rpb_table(np.asarray(params["na_rpb"])),
        ssm_conv_w=np.ascontiguousarray(params["ssm_conv_w"]).reshape(L, 5, 12, 128),
        ssm_conv_b=np.ascontiguousarray(params["ssm_conv_b"]).reshape(L, 12, 128),
        ssm_dt_bias=np.ascontiguousarray(params["ssm_dt_bias"]).reshape(L, 32),
        ssm_a_log=np.ascontiguousarray(params["ssm_a_log"]).reshape(L, 32),
        ssm_d_exp=np.ascontiguousarray(np.repeat(np.asarray(params["ssm_d"]), 64, axis=-1)).reshape(L, 8, 128),
        ssm_norm_w=np.ascontiguousarray(params["ssm_norm_w"]).reshape(L, 8, 128),
        tri_u=(t[:, None] <= t[None, :]).astype(np.float32), tri_l=(t[:, None] >= t[None, :]).astype(np.float32),
        sel32=sel.reshape(32, 32 * 128),
        hgrn_lb_logits=np.ascontiguousarray(params["hgrn_lb_logits"]).reshape(4, 8, 128),
        hgrn_norm_w=np.ascontiguousarray(params["hgrn_norm_w"]).reshape(L, 8, 128),
        mask32u=m32u, mask32l=m32l,
    )


class MixProg(Prog):
    def declare_mixer_inputs(self):
        L = self.L
        self.gqa_qw = self.inp("gqa_q_norm_w", [L, 128])
        self.gqa_kw = self.inp("gqa_k_norm_w", [L, 128])
        self.rope_cos = self.inp("rope_cos", [128, NLAT])
        self.rope_sin = self.inp("rope_sin", [128, NLAT])
        self.rotT_d = self.inp("rotT", [128, 128])
        self.exch_d = self.inp("exch", [128, 128])
        self.na_mask_d = self.inp("na_mask", [4, 8, 128, 512])
        self.na_rpbF = self.inp("na_rpbF", [L, 8, 23, 127])
        self.ssm_conv_w = self.inp("ssm_conv_w", [L, 5, 12, 128])
        self.ssm_conv_b = self.inp("ssm_conv_b", [L, 12, 128])
        self.ssm_dt_bias = self.inp("ssm_dt_bias", [L, 32])
        self.ssm_a_log = self.inp("ssm_a_log", [L, 32])
        self.ssm_d_exp = self.inp("ssm_d_exp", [L, 8, 128])
        self.ssm_norm_w = self.inp("ssm_norm_w", [L, 8, 128])
        self.tri_u_d = self.inp("tri_u", [128, 128])
        self.tri_l_d = self.inp("tri_l", [128, 128])
        self.sel32_d = self.inp("sel32", [32, 32 * 128])
        self.hg_lb = self.inp("hgrn_lb_logits", [4, 8, 128])
        self.hg_nw = self.inp("hgrn_norm_w", [L, 8, 128])
        self.mask32u_d = self.inp("mask32u", [128, 128])
        self.mask32l_d = self.inp("mask32l", [128, 128])

    def declare_mixer_scratch(self):
        self.XS_F = self.scratch("XS_F", [1024, NT], F32)
        self.XS_T = self.scratch("XS_T", [NT, 1024], BF16)

    def stage_mixers(self, l):
        if "D" in self.mixers:
            self.stage_gqa(l)
        if "B" in self.mixers:
            self.stage_na(l)
        if "C" in self.mixers:
            self.stage_ssd(l)
        if "A" in self.mixers:
            self.stage_hgrn(l)

    def attn_core(self, q_ap, qb, nq, keys, vfn, out_dst, extra=None):
        S = self.S
        O, Ob = self.psum("acc")
        Dn, Dnb = self.psum("acc")
        nk = len(keys)
        for i, (kT, kb, vkey) in enumerate(keys):
            sp, spb = self.psum("s")
            ex = extra(i) if extra else []
            S.op("pe", "matmul", sp[:, 0:nq], lhsT=kT, rhs=q_ap, start=True, stop=(len(ex) == 0),
                 reads=[kb, qb], writes=[spb], signal=(len(ex) == 0))
            for j, (lt, ltb, rh, rhb) in enumerate(ex):
                last = (j == len(ex) - 1)
                S.op("pe", "matmul", sp[:, 0:nq], lhsT=lt, rhs=rh, start=False, stop=last,
                     reads=[ltb, rhb], writes=[spb], signal=last)
            pt, ptb = self.pts[self.pt_i % len(self.pts)]
            self.pt_i += 1
            S.op("act", "activation", out=pt[:, 0:nq], in_=sp[:, 0:nq], func=AF.Exp, reads=[spb], writes=[ptb])
            v, vb = vfn(vkey)
            S.op("pe", "matmul", O[:, 0:nq], lhsT=v, rhs=pt[:, 0:nq], start=(i == 0), stop=(i == nk - 1),
                 reads=[vb, ptb], writes=[Ob], signal=(i == nk - 1))
            S.op("pe", "matmul", Dn[:, 0:nq], lhsT=self.onesb, rhs=pt[:, 0:nq], start=(i == 0), stop=(i == nk - 1),
                 reads=[self.onesb_b, ptb], writes=[Dnb], signal=(i == nk - 1))
        rc, rcb = self.recs[self.rec_i % 2]
        ob, obb = self.obs[self.rec_i % 2]
        self.rec_i += 1
        S.op("dve", "reciprocal", out=rc[:, 0:nq], in_=Dn[:, 0:nq], reads=[Dnb], writes=[rcb])
        S.op("dve", "tensor_tensor", out=ob[:, 0:nq], in0=O[:, 0:nq], in1=rc[:, 0:nq], op=ALU.mult,
             reads=[Ob, rcb], writes=[obb])
        S.dma("sp", out_dst, ob[:, 0:nq], reads=[obb])

    def attn_bufs(self):
        A = self.A
        self.pts = [A.alloc([128, 512], BF16, "pt%d" % i) for i in range(3)]
        self.recs = [A.alloc([128, 512], F32, "rec%d" % i) for i in range(2)]
        self.obs = [A.alloc([128, 512], BF16, "ob%d" % i) for i in range(2)]
        self.pt_i = 0
        self.rec_i = 0

    def stage_gqa(self, l):
        S, A = self.S, self.A
        A.push()
        cos, cosb = A.alloc([128, NLAT], F32, "cos")
        sin, sinb = A.alloc([128, NLAT], F32, "sin")
        S.dma("sp", cos, self.rope_cos, writes=[cosb])
        S.dma("sp", sin, self.rope_sin, writes=[sinb])
        rt32, rt32b = A.alloc([128, 128], F32, "rt32")
        S.dma("sp", rt32, self.rotT_d, writes=[rt32b])
        rt, rtb = A.alloc([128, 128], BF16, "rt")
        S.op("dve", "tensor_copy", out=rt, in_=rt32, reads=[rt32b], writes=[rtb])
        wq, wqb = A.alloc([128, 2], F32, "wq")
        wk, wkb = A.alloc([128, 2], F32, "wk")
        S.dma("sp", wq[:, 0:1], self.gqa_qw[l].rearrange("(p o) -> p o", o=1), writes=[wqb])
        S.dma("sp", wk[:, 0:1], self.gqa_kw[l].rearrange("(p o) -> p o", o=1), writes=[wkb])
        S.op("dve", "tensor_scalar", out=wq[:, 1:2], in0=wq[:, 0:1], scalar1=float(HD ** -0.5), scalar2=None, op0=ALU.mult,
             reads=[wqb], writes=[wqb])
        QT = [A.alloc([128, NT], BF16, "gq%d" % h) for h in range(8)]
        KT = [A.alloc([128, NT], BF16, "gk%d" % h) for h in range(2)]
        V, Vb = A.alloc([128, NTILE, 256], BF16, "gv")
        S.dma("sp", V, self.PS["DV"].rearrange("(t p) c -> p t c", p=128), writes=[Vb])
        A.push()
        xs = [A.alloc([128, NT], F32, "gx%d" % i) for i in range(2)]
        sq, sqb = A.alloc([128, NT], BF16, "gsq")
        rs, rsb = A.alloc([128, NT], F32, "grs")
        xn, xnb = A.alloc([128, NT], F32, "gxn")
        xb, xbb = A.alloc([128, NLAT], BF16, "gxb")
        t1, t1b = A.alloc([128, NLAT], F32, "gt1")
        t2s = [A.alloc([128, 512], F32, "gt2%d" % i) for i in range(2)]
        chunks = [(t0, min(512, NT - t0)) for t0 in range(0, NT, 512)]
        for hi in range(10):
            isq = hi < 8
            src = self.PS["DQ"][hi * 128:(hi + 1) * 128, :] if isq else self.PS["DK"][(hi - 8) * 128:(hi - 7) * 128, :]
            dst, dstb = QT[hi] if isq else KT[hi - 8]
            wcol = wq[:, 1:2] if isq else wk[:, 0:1]
            wcolb = wqb if isq else wkb
            x, xb_ = xs[hi % 2]
            S.dma("sp", x, src, writes=[xb_])
            S.op("act", "activation", out=sq, in_=x, func=AF.Square, reads=[xb_], writes=[sqb])
            for (t0, nt) in chunks:
                ps, pb = self.psum()
                S.op("pe", "matmul", ps[:, 0:nt], lhsT=self.onesb, rhs=sq[:, t0:t0 + nt], start=True, stop=True,
                     reads=[self.onesb_b, sqb], writes=[pb])
                S.op("act", "activation", out=rs[:, t0:t0 + nt], in_=ps[:, 0:nt], func=AF.Sqrt, bias=self.eps_t, scale=1.0 / HD,
                     reads=[pb, self.eps_b], writes=[rsb])
            S.op("dve", "reciprocal", out=rs, in_=rs, reads=[rsb], writes=[rsb])
            S.op("dve", "scalar_tensor_tensor", out=xn, in0=x, scalar=wcol, in1=rs, op0=ALU.mult, op1=ALU.mult,
                 reads=[xb_, wcolb, rsb], writes=[xnb])
            S.op("pool", "tensor_copy", out=dst[:, 0:NCTX], in_=xn[:, 0:NCTX], reads=[xnb], writes=[dstb])
            S.op("pool", "tensor_copy", out=xb, in_=xn[:, NCTX:NT], reads=[xnb], writes=[xbb])
            S.op("pool", "tensor_tensor", out=t1, in0=xn[:, NCTX:NT], in1=cos, op=ALU.mult, reads=[xnb, cosb], writes=[t1b])
            for ci in range(4):
                ps, pb = self.psum()
                S.op("pe", "matmul", ps[:, :], lhsT=rt, rhs=xb[:, ci * 512:(ci + 1) * 512], start=True, stop=True,
                     reads=[rtb, xbb], writes=[pb])
                t2, t2b = t2s[ci % 2]
                S.op("dve", "tensor_tensor", out=t2, in0=ps[:, :], in1=sin[:, ci * 512:(ci + 1) * 512], op=ALU.mult,
                     reads=[pb, sinb], writes=[t2b])
                S.op("pool", "tensor_tensor", out=dst[:, NCTX + ci * 512:NCTX + (ci + 1) * 512], in0=t1[:, ci * 512:(ci + 1) * 512],
                     in1=t2, op=ALU.add, reads=[t1b, t2b], writes=[dstb])
        A.pop()
        A.push()
        self.attn_bufs()
        for hq in range(8):
            kv = hq // 4
            q, qb = QT[hq]
            k, kb = KT[kv]
            vfn = lambda kt, kv=kv: (V[:, kt, kv * 128:(kv + 1) * 128], Vb)
            row0 = 3072 + hq * 128
            keys = [(k[:, kt * 128:(kt + 1) * 128], kb, kt) for kt in range(2)]
            self.attn_core(q[:, 0:NCTX], qb, NCTX, keys, vfn, self.MIXT[row0:row0 + 128, 0:NCTX])
            keys = [(k[:, kt * 128:(kt + 1) * 128], kb, kt) for kt in range(NTILE)]
            for qc in range(4):
                c0 = NCTX + qc * 512
                self.attn_core(q[:, c0:c0 + 512], qb, 512, keys, vfn, self.MIXT[row0:row0 + 128, c0:c0 + 512])
        A.pop()
        A.pop()
        S.barrier()

    def stage_na(self, l):
        S, A = self.S, self.A
        A.push()
        ex32, ex32b = A.alloc([128, 128], F32, "ex32")
        S.dma("sp", ex32, self.exch_d, writes=[ex32b])
        ex, exb = A.alloc([128, 128], BF16, "ex")
        S.op("dve", "tensor_copy", out=ex, in_=ex32, reads=[ex32b], writes=[exb])
        masks = {}
        mst = [A.alloc([128, 512], F32, "mst%d" % i) for i in range(2)]
        k = 0
        for ci in range(4):
            for ti, kr0 in enumerate(na_chunk_tiles(ci)):
                m, mb = A.alloc([128, 512], BF16, "nm%d_%d" % (ci, ti))
                st, stb = mst[k % 2]; k += 1
                S.dma("sp", st, self.na_mask_d[ci, ti], writes=[stb])
                S.op("pool", "tensor_copy", out=m, in_=st, reads=[stb], writes=[mb])
                masks[(ci, kr0)] = (m, mb)
        self.attn_bufs()
        qs = [A.alloc([128, NT], BF16, "nq%d" % i) for i in range(2)]
        ks = [A.alloc([128, NT], BF16, "nk%d" % i) for i in range(2)]
        vs = [A.alloc([128, NTILE, 128], BF16, "nv%d" % i) for i in range(2)]
        bst = [(A.alloc([128, 512], F32, "bst%d" % i), Buf("bstA%d" % i), Buf("bstB%d" % i)) for i in range(2)]
        bts = [[A.alloc([128, 512], BF16, "nb%d_%d" % (i, d)) for d in range(8)] for i in range(2)]
        rpb_t = self.na_rpbF.tensor
        for h in range(8):
            q, qb = qs[h % 2]
            kk, kb = ks[h % 2]
            v, vb = vs[h % 2]
            S.dma("sp", q, self.PS["BQ"][h * 128:(h + 1) * 128, :], writes=[qb])
            S.dma("sp", kk, self.PS["BK"][h * 128:(h + 1) * 128, :], writes=[kb])
            S.dma("sp", v, self.PS["BV"][:, h * 128:(h + 1) * 128].rearrange("(t p) c -> p t c", p=128), writes=[vb])
            bias = {}
            for di, dl in enumerate(NA_DELTAS):
                bt, btb = bts[h % 2][di]
                (stg, stg0), hb0, hb1 = bst[di % 2]
                for a, hb in ((0, hb0), (1, hb1)):
                    off = ((l * 8 + h) * 23 + (10 - dl) + a) * 127
                    src = bass.AP(tensor=rpb_t, offset=off, ap=[[1, 64], [127, 8], [1, 64]])
                    S.dma("sp", stg[a * 64:(a + 1) * 64, :].rearrange("p (b c) -> p b c", b=8), src, writes=[hb])
                S.op("pool", "tensor_copy", out=bt, in_=stg, reads=[hb0, hb1], writes=[btb, stg0])
                bias[dl] = (bt, btb)
            vfn = lambda kt: (v[:, kt, :], vb)
            row0 = 1024 + h * 128
            keys = [(kk[:, kt * 128:(kt + 1) * 128], kb, kt) for kt in range(2)]
            self.attn_core(q[:, 0:NCTX], qb, NCTX, keys, vfn, self.MIXT[row0:row0 + 128, 0:NCTX])
            for ci in range(4):
                c0 = NCTX + ci * 512
                tiles = na_chunk_tiles(ci)
                keys = []
                exl = []
                for kr0 in tiles:
                    kt = 2 + kr0 // 2
                    keys.append((kk[:, kt * 128:(kt + 1) * 128], kb, kt))
                    bt, btb = bias[kr0 - 8 * ci]
                    m, mb = masks[(ci, kr0)]
                    exl.append([(ex, exb, bt, btb), (ex, exb, m, mb)])
                for kt in range(2):
                    keys.append((kk[:, kt * 128:(kt + 1) * 128], kb, kt))
                    exl.append([])
                self.attn_core(q[:, c0:c0 + 512], qb, 512, keys, vfn, self.MIXT[row0:row0 + 128, c0:c0 + 512],
                               extra=lambda i, exl=exl: exl[i])
        A.pop()
        S.barrier()

    def stage_ssd(self, l):
        S, A = self.S, self.A
        A.push()
        triu, triub = A.alloc([128, 128], F32, "triu")
        tril, trilb = A.alloc([128, 128], F32, "tril")
        S.dma("sp", triu, self.tri_u_d, writes=[triub])
        S.dma("sp", tril, self.tri_l_d, writes=[trilb])
        negs = []
        for (t_, tb_) in ((triu, triub), (tril, trilb)):
            n_, nb_ = A.alloc([128, 128], F32, "negm")
            S.op("dve", "tensor_scalar", out=n_, in0=t_, scalar1=-1.0, scalar2=-NEG, op0=ALU.add, op1=ALU.mult,
                 reads=[tb_], writes=[nb_])
            negs.append((n_, nb_))
        sel, selb = A.alloc([128, 32 * 128], F32, "sel32")
        S.dma("sp", sel[0:32, :], self.sel32_d, writes=[selb])
        ones32, ones32b = A.alloc([128, 128], F32, "ones32")
        S.op("pool", "memset", ones32, 1.0, writes=[ones32b])
        cw, cwb = A.alloc([128, 5, 12], F32, "cw")
        for j in range(5):
            self.vec_to_fm(self.ssm_conv_w[l, j], 12, cw[:, j, :], cwb)
        cb, cbb = A.alloc([128, 12], F32, "cb")
        self.vec_to_fm(self.ssm_conv_b[l], 12, cb, cbb)
        dcol, dcolb = A.alloc([128, 8], F32, "dcol")
        self.vec_to_fm(self.ssm_d_exp[l], 8, dcol, dcolb)
        nw, nwb = A.alloc([128, 8], F32, "snw")
        self.vec_to_fm(self.ssm_norm_w[l], 8, nw, nwb)
        dtb, dtbb = A.alloc([128, 32], F32, "dtb")
        S.dma("sp", dtb, self.ssm_dt_bias[l].partition_broadcast(128), writes=[dtbb])
        abc, abcb = A.alloc([128, 32], F32, "abc")
        S.dma("sp", abc, self.ssm_a_log[l].partition_broadcast(128), writes=[abcb])
        S.op("act", "activation", out=abc, in_=abc, func=AF.Exp, reads=[abcb], writes=[abcb])
        S.op("dve", "tensor_scalar", out=abc, in0=abc, scalar1=-1.0, scalar2=None, op0=ALU.mult, reads=[abcb], writes=[abcb])
        one_t, one_tb = A.alloc([128, 1], F32, "one")
        S.op("pool", "memset", one_t, 1.0, writes=[one_tb])
        YT, YTb = A.alloc([128, 8, NT], F32, "YT")
        A.push()
        BT, BTb = A.alloc([128, 2, NT], BF16, "BT")
        CT, CTb = A.alloc([128, 2, NT], BF16, "CT")
        Btok, Btokb = A.alloc([128, NTILE, 256], BF16, "Btok")
        A.push()
        us = [A.alloc([128, NT], F32, "cu%d" % i) for i in range(2)]
        accs = [A.alloc([128, NT], F32, "cacc%d" % i) for i in range(2)]
        xbt, xbtb = A.alloc([128, NT], BF16, "cxb")
        stg = [A.alloc([128, 4, 128], BF16, "cstg%d" % i) for i in range(2)]
        sgi = 0
        for ct in range(12):
            u, ub = us[ct % 2]
            acc, accb = accs[ct % 2]
            S.dma("sp", u, self.PS["CX"][ct * 128:(ct + 1) * 128, :], writes=[ub])
            S.op("act", "activation", out=acc, in_=u, func=AF.Identity, scale=cw[:, 2, ct:ct + 1], bias=cb[:, ct:ct + 1],
                 reads=[ub, cwb, cbb], writes=[accb])
            for j in (0, 1, 3, 4):
                s_ = j - 2
                for (a_, b_) in ((0, NCTX), (NCTX, NT)):
                    lo, hi = max(a_, a_ - s_), min(b_, b_ - s_)
                    S.op("dve", "scalar_tensor_tensor", out=acc[:, lo:hi], in0=u[:, lo + s_:hi + s_], scalar=cw[:, j, ct:ct + 1],
                         in1=acc[:, lo:hi], op0=ALU.mult, op1=ALU.add, reads=[ub, cwb, accb], writes=[accb])
            S.op("act", "activation", out=acc, in_=acc, func=AF.Silu, reads=[accb], writes=[accb])
            if ct < 8:
                S.dma("sp", self.XS_F[ct * 128:(ct + 1) * 128, :], acc, reads=[accb])
                xb_, xbb_ = xbt, xbtb
            elif ct < 10:
                xb_, xbb_ = BT[:, ct - 8, :], BTb
            else:
                xb_, xbb_ = CT[:, ct - 10, :], CTb
            S.op("pool", "tensor_copy", out=xb_, in_=acc, reads=[accb], writes=[xbb_])
            if ct < 10:
                for t0 in range(0, NTILE, 4):
                    n4 = min(4, NTILE - t0)
                    ps, pb = self.psum()
                    psb16 = ps[:, :].bitcast(BF16)
                    for q in range(n4):
                        S.op("pe", "transpose", out=psb16[:, q * 128:(q + 1) * 128], in_=xb_[:, (t0 + q) * 128:(t0 + q + 1) * 128],
                             identity=self.identb, reads=[xbb_, self.identb_b], writes=[pb], signal=(q == n4 - 1))
                    if ct < 8:
                        sg, sgb = stg[sgi % 2]; sgi += 1
                        S.op("dve", "tensor_copy", out=sg[:, 0:n4, :], in_=psb16[:, 0:n4 * 128].rearrange("p (t c) -> p t c", t=n4),
                             reads=[pb], writes=[sgb])
                        S.dma("sp", self.XS_T[t0 * 128:(t0 + n4) * 128, ct * 128:(ct + 1) * 128].rearrange("(t p) c -> p t c", p=128),
                              sg[:, 0:n4, :], reads=[sgb])
                    else:
                        S.op("dve", "tensor_copy", out=Btok[:, t0:t0 + n4, (ct - 8) * 128:(ct - 7) * 128],
                             in_=psb16[:, 0:n4 * 128].rearrange("p (t c) -> p t c", t=n4), reads=[pb], writes=[Btokb])
        A.pop()
        dt, dtb_ = A.alloc([128, NTILE, 32], F32, "dt")
        S.dma("sp", dt, self.PS["CDT"].rearrange("(t p) c -> p t c", p=128), writes=[dtb_])
        S.op("dve", "tensor_tensor", out=dt, in0=dt, in1=dtb.unsqueeze(1).to_broadcast([128, NTILE, 32]), op=ALU.add,
             reads=[dtb_, dtbb], writes=[dtb_])
        S.op("act", "activation", out=dt, in_=dt, func=AF.Exp, reads=[dtb_], writes=[dtb_])
        S.op("act", "activation", out=dt, in_=dt, func=AF.Ln, bias=one_t, scale=1.0, reads=[dtb_, one_tb], writes=[dtb_])
        dta, dtab = A.alloc([128, NTILE, 32], F32, "dta")
        S.op("dve", "tensor_tensor", out=dta, in0=dt, in1=abc.unsqueeze(1).to_broadcast([128, NTILE, 32]), op=ALU.mult,
             reads=[dtb_, abcb], writes=[dtab])
        cs, csb = A.alloc([128, NTILE, 32], F32, "cs")
        ncs, ncsb = A.alloc([128, NTILE, 32], F32, "ncs")
        wdec, wdecb = A.alloc([128, NTILE, 32], F32, "wdec")
        ach, achb = A.alloc([128, NTILE, 32], F32, "ach")
        csT, csTb = A.alloc([128, NTILE, 128], F32, "csT")
        for t in range(NTILE):
            ps, pb = self.psum()
            S.op("pe", "matmul", ps[:, 0:16], lhsT=triu, rhs=dta[:, t, 0:16], start=True, stop=True,
                 reads=[triub, dtab], writes=[pb], signal=False)
            S.op("pe", "matmul", ps[:, 16:32], lhsT=tril, rhs=dta[:, t, 16:32], start=True, stop=True,
                 reads=[trilb, dtab], writes=[pb], signal=False)
            S.op("pe", "matmul", ps[:, 32:64], lhsT=ones32, rhs=dta[:, t, :], start=True, stop=True,
                 reads=[ones32b, dtab], writes=[pb])
            S.op("dve", "tensor_copy", out=cs[:, t, :], in_=ps[:, 0:32], reads=[pb], writes=[csb])
            S.op("dve", "tensor_tensor", out=wdec[:, t, :], in0=ps[:, 32:64], in1=cs[:, t, :], op=ALU.subtract,
                 reads=[pb, csb], writes=[wdecb])
            S.op("dve", "tensor_copy", out=ach[:, t, :], in_=ps[:, 32:64], reads=[pb], writes=[achb])
            ps2, pb2 = self.psum()
            S.op("pe", "transpose", out=ps2[0:32, 0:128], in_=cs[:, t, :], identity=self.ident, reads=[csb, self.ident_b], writes=[pb2])
            S.op("dve", "tensor_copy", out=csT[0:32, t, :], in_=ps2[0:32, 0:128], reads=[pb2], writes=[csTb])
        S.op("dve", "tensor_scalar", out=ncs, in0=cs, scalar1=-1.0, scalar2=None, op0=ALU.mult, reads=[csb], writes=[ncsb])
        S.op("act", "activation", out=wdec, in_=wdec, func=AF.Exp, reads=[wdecb], writes=[wdecb])
        S.op("dve", "tensor_tensor", out=wdec, in0=wdec, in1=dt, op=ALU.mult, reads=[wdecb, dtb_], writes=[wdecb])
        S.op("act", "activation", out=ach, in_=ach, func=AF.Exp, reads=[achb], writes=[achb])
        CBT, CBTb = A.alloc([128, 2, NTILE, 128], BF16, "CBT")
        for g in range(2):
            for t in range(NTILE):
                ps, pb = self.psum()
                S.op("pe", "matmul", ps[:, 0:128], lhsT=BT[:, g, t * 128:(t + 1) * 128], rhs=CT[:, g, t * 128:(t + 1) * 128],
                     start=True, stop=True, reads=[BTb, CTb], writes=[pb])
                self.copy(self.alt(), CBT[:, g, t, :], ps[:, 0:128], [pb], [CBTb])
        H, Hb_ = A.alloc([128, 2, 512], F32, "H")
        Hh, Hhb = A.alloc([128, 2, 512], BF16, "Hh")
        xts = [A.alloc([128, 1024], BF16, "sxt%d" % i) for i in range(2)]
        ers = [A.alloc([128, 128], F32, "er%d" % i) for i in range(2)]
        es = [A.alloc([128, 128], F32, "e%d" % i) for i in range(2)]
        chs = [A.alloc([128, 128], BF16, "ch%d" % i) for i in range(2)]
        mts = [A.alloc([128, 128], BF16, "mt%d" % i) for i in range(2)]
        xhs = [A.alloc([128, 512], BF16, "xh%d" % i) for i in range(2)]
        it = 0
        xi = 0
        for d in range(2):
            order = list(range(NTILE)) if d == 0 else [1, 0] + list(range(NTILE - 1, 1, -1))
            neg, negb = negs[d]
            S.op("pool", "memset", H, 0.0, writes=[Hb_])
            S.op("pool", "memset", Hh, 0.0, writes=[Hhb])
            for t in order:
                xt, xtb = xts[xi % 2]; xi += 1
                S.dma("sp", xt, self.XS_T[t * 128:(t + 1) * 128, :], writes=[xtb])
                for h in range(16):
                    c = d * 16 + h
                    g = h // 8
                    er, erb = ers[it % 2]; e_, eb_ = es[it % 2]; ch, chb = chs[it % 2]; mt, mtb = mts[it % 2]
                    it += 1
                    r1, r1b = self.psum("s")
                    S.op("pe", "matmul", r1[:, 0:128], lhsT=sel[0:32, c * 128:(c + 1) * 128], rhs=csT[0:32, t, :], start=True, stop=True,
                         reads=[selb, csTb], writes=[r1b])
                    r2, r2b = self.psum("s")
                    S.op("pe", "matmul", r2[:, 0:128], lhsT=sel[0:32, c * 128:(c + 1) * 128], rhs=csT[0:32, t, :], start=True, stop=False,
                         reads=[selb, csTb], writes=[r2b], signal=False)
                    S.op("pe", "matmul", r2[:, 0:128], lhsT=self.ident, rhs=neg, start=False, stop=True,
                         reads=[self.ident_b, negb], writes=[r2b])
                    S.op("act", "activation", out=er, in_=r1[:, 0:128], func=AF.Exp, reads=[r1b], writes=[erb])
                    S.op("act", "activation", out=e_, in_=r2[:, 0:128], func=AF.Exp, bias=ncs[:, t, c:c + 1], scale=1.0,
                         reads=[r2b, ncsb], writes=[eb_])
                    S.op("dve", "tensor_tensor", out=ch, in0=CT[:, g, t * 128:(t + 1) * 128], in1=er, op=ALU.mult,
                         reads=[CTb, erb], writes=[chb])
                    S.op("dve", "scalar_tensor_tensor", out=mt, in0=e_, scalar=dt[:, t, c:c + 1], in1=CBT[:, g, t, :],
                         op0=ALU.mult, op1=ALU.mult, reads=[eb_, dtb_, CBTb], writes=[mtb])
                    y, yb = self.psum("acc")
                    hp = (h // 2) * 128
                    hl = ((h % 8) // 2) * 128
                    S.op("pe", "matmul", y[:, 0:128], lhsT=xt[:, hp:hp + 128], rhs=mt, start=True, stop=False,
                         reads=[xtb, mtb], writes=[yb], signal=False)
                    S.op("pe", "matmul", y[:, 0:128], lhsT=Hh[:, g, hl:hl + 128], rhs=ch, start=False, stop=True,
                         reads=[Hhb, chb], writes=[yb])
                    p0 = (h % 2) * 64
                    ydst = YT[p0:p0 + 64, h // 2, t * 128:(t + 1) * 128]
                    if d == 0:
                        S.op("dve", "tensor_copy", out=ydst, in_=y[p0:p0 + 64, 0:128], reads=[yb], writes=[YTb])
                    else:
                        S.op("dve", "tensor_tensor", out=ydst, in0=y[p0:p0 + 64, 0:128], in1=ydst, op=ALU.add,
                             reads=[yb, YTb], writes=[YTb])
                for g in range(2):
                    xh, xhb = xhs[g]
                    c0 = d * 16 + g * 8
                    S.op("dve", "tensor_tensor", out=xh.rearrange("p (h c) -> p h c", h=8),
                         in0=xt[:, g * 512:(g + 1) * 512].rearrange("p (h c) -> p h c", h=8),
                         in1=wdec[:, t, c0:c0 + 8].unsqueeze(2).to_broadcast([128, 8, 64]), op=ALU.mult,
                         reads=[xtb, wdecb], writes=[xhb])
                    u_, u_b = self.psum("acc")
                    S.op("pe", "matmul", u_[:, :], lhsT=Btok[:, t, g * 128:(g + 1) * 128], rhs=xh, start=True, stop=True,
                         reads=[Btokb, xhb], writes=[u_b])
                    Hg = H[:, g, :].rearrange("p (h c) -> p h c", h=8)
                    S.op("dve", "tensor_tensor", out=Hg, in0=Hg, in1=ach[:, t, c0:c0 + 8].unsqueeze(2).to_broadcast([128, 8, 64]),
                         op=ALU.mult, reads=[Hb_, achb], writes=[Hb_])
                    S.op("dve", "tensor_tensor", out=H[:, g, :], in0=H[:, g, :], in1=u_[:, :], op=ALU.add,
                         reads=[Hb_, u_b], writes=[Hb_])
                    S.op("pool", "tensor_copy", out=Hh[:, g, :], in_=H[:, g, :], reads=[Hb_], writes=[Hhb])
        A.pop()
        zs = [A.alloc([128, NT], F32, "sz%d" % i) for i in range(2)]
        sq, sqb = A.alloc([128, NT], BF16, "ssq")
        rs, rsb = A.alloc([128, NT], F32, "srs")
        obs = [A.alloc([128, NT], BF16, "sob%d" % i) for i in range(2)]
        chunks = [(t0, min(512, NT - t0)) for t0 in range(0, NT, 512)]
        zi = 0
        for g in range(2):
            for q in range(4):
                ct = g * 4 + q
                z, zb = zs[zi % 2]; zi += 1
                S.dma("sp", z, self.XS_F[ct * 128:(ct + 1) * 128, :], writes=[zb])
                S.op("dve", "scalar_tensor_tensor", out=YT[:, ct, :], in0=z, scalar=dcol[:, ct:ct + 1], in1=YT[:, ct, :],
                     op0=ALU.mult, op1=ALU.add, reads=[zb, dcolb, YTb], writes=[YTb])
                z2, z2b = zs[zi % 2]; zi += 1
                S.dma("sp", z2, self.PS["CZ"][ct * 128:(ct + 1) * 128, :], writes=[z2b])
                S.op("act", "activation", out=z2, in_=z2, func=AF.Silu, reads=[z2b], writes=[z2b])
                S.op("dve", "tensor_tensor", out=YT[:, ct, :], in0=YT[:, ct, :], in1=z2, op=ALU.mult, reads=[YTb, z2b], writes=[YTb])
            pss = [self.psum() for _ in chunks]
            for q in range(4):
                ct = g * 4 + q
                S.op("act", "activation", out=sq, in_=YT[:, ct, :], func=AF.Square, reads=[YTb], writes=[sqb])
                for (ps, pb), (t0, nt) in zip(pss, chunks):
                    S.op("pe", "matmul", ps[:, 0:nt], lhsT=self.onesb, rhs=sq[:, t0:t0 + nt], start=(q == 0), stop=(q == 3),
                         reads=[self.onesb_b, sqb], writes=[pb], signal=True)
            for (ps, pb), (t0, nt) in zip(pss, chunks):
                S.op("act", "activation", out=rs[:, t0:t0 + nt], in_=ps[:, 0:nt], func=AF.Sqrt, bias=self.eps_t, scale=1.0 / 512,
                     reads=[pb, self.eps_b], writes=[rsb])
            S.op("dve", "reciprocal", out=rs, in_=rs, reads=[rsb], writes=[rsb])
            for q in range(4):
                ct = g * 4 + q
                ob, obb = obs[q % 2]
                S.op("dve", "scalar_tensor_tensor", out=ob, in0=YT[:, ct, :], scalar=nw[:, ct:ct + 1], in1=rs, op0=ALU.mult, op1=ALU.mult,
                     reads=[YTb, nwb, rsb], writes=[obb])
                S.dma("sp", self.MIXT[2048 + ct * 128:2048 + (ct + 1) * 128, :], ob, reads=[obb])
        A.pop()
        S.barrier()

    def stage_hgrn(self, l):
        S, A = self.S, self.A
        A.push()
        lg, lgb = A.alloc([128, 4, 8], F32, "hlg")
        for lp in range(4):
            self.vec_to_fm(self.hg_lb[lp], 8, lg[:, lp, :], lgb)
        S.op("act", "activation", out=lg, in_=lg, func=AF.Exp, reads=[lgb], writes=[lgb])
        lbt, lbb = A.alloc([128, 4, 8], F32, "hlb")
        S.op("dve", "tensor_tensor", out=lbt[:, 0, :], in0=lg[:, 0, :], in1=lg[:, 1, :], op=ALU.add, reads=[lgb], writes=[lbb])
        S.op("dve", "tensor_tensor", out=lbt[:, 0, :], in0=lbt[:, 0, :], in1=lg[:, 2, :], op=ALU.add, reads=[lgb, lbb], writes=[lbb])
        S.op("dve", "tensor_tensor", out=lbt[:, 0, :], in0=lbt[:, 0, :], in1=lg[:, 3, :], op=ALU.add, reads=[lgb, lbb], writes=[lbb])
        S.op("pool", "memset", lbt[:, 1, :], 0.0, writes=[lbb])
        for lp in range(1, l + 1):
            S.op("dve", "tensor_tensor", out=lbt[:, 1, :], in0=lbt[:, 1, :], in1=lg[:, lp, :], op=ALU.add, reads=[lgb, lbb], writes=[lbb])
        S.op("dve", "reciprocal", out=lbt[:, 3, :], in_=lbt[:, 0, :], reads=[lbb], writes=[lbb])
        S.op("dve", "tensor_tensor", out=lbt[:, 1, :], in0=lbt[:, 1, :], in1=lbt[:, 3, :], op=ALU.mult, reads=[lbb], writes=[lbb])
        S.op("dve", "tensor_scalar", out=lbt[:, 2, :], in0=lbt[:, 1, :], scalar1=-1.0, scalar2=1.0, op0=ALU.mult, op1=ALU.add,
             reads=[lbb], writes=[lbb])
        nw, nwb = A.alloc([128, 8], F32, "hnw")
        self.vec_to_fm(self.hg_nw[l], 8, nw, nwb)
        m32 = []
        for src in (self.mask32u_d, self.mask32l_d):
            m_, mb_ = A.alloc([128, 128], F32, "m32")
            S.dma("sp", m_, src, writes=[mb_])
            m32.append((m_, mb_))
        rms_ = []
        for d in range(2):
            r_, rb_ = A.alloc([128, NT], BF16, "rmask%d" % d)
            S.op("pool", "memset", r_, 1.0, writes=[rb_])
            r3 = r_.rearrange("p (n c) -> p n c", c=32)
            S.op("pool", "memset", r3[:, :, 0:1] if d == 0 else r3[:, :, 31:32], 0.0, writes=[rb_])
            rms_.append((r_, rb_))
        V, Vb = A.alloc([128, NTILE, 128], BF16, "hV")
        vm, vmb = A.alloc([128, NTILE, 4, 128], BF16, "hvm")
        sq_, sqb_ = A.alloc([128, NT], F32, "hsq")
        a1, a1b = A.alloc([128, NT], F32, "ha1")
        a2, a2b = A.alloc([128, NT], F32, "ha2")
        a3, a3b = A.alloc([128, NT], F32, "ha3")
        qt, qtb = A.alloc([128, NT], BF16, "hqt")
        kt, ktb = A.alloc([128, NT], BF16, "hkt")
        OT, OTb = A.alloc([128, NT], F32, "hOT")
        ktoks = [A.alloc([128, 128], BF16, "hktok%d" % i) for i in range(2)]
        sms = [A.alloc([128, 128], BF16, "hsm%d" % i) for i in range(2)]
        Ts = [A.alloc([128, 128], F32, "hT%d" % i) for i in range(2)]
        Sbs = [A.alloc([128, 128], BF16, "hSb%d" % i) for i in range(2)]
        sqb2, sqb2b = A.alloc([128, NT], BF16, "hsq2")
        ob, obb = A.alloc([128, NT], BF16, "hob")
        chunks = [(t0, min(512, NT - t0)) for t0 in range(0, NT, 512)]
        segs = ("AFF", "AFB")
        ki = 0
        for h in range(8):
            rows = slice(h * 128, (h + 1) * 128)
            S.dma("sp", V, self.PS["AI"][:, rows].rearrange("(t p) c -> p t c", p=128), writes=[Vb])
            for n in range(4):
                S.op("pool", "tensor_scalar", out=vm[:, :, n, :], in0=V, scalar1=m32[0][0][:, 32 * n + 31:32 * n + 32], scalar2=None,
                     op0=ALU.mult, reads=[Vb, m32[0][1]], writes=[vmb])
            S.dma("sp", sq_, self.PS["AQ"][rows, :], writes=[sqb_])
            S.op("act", "activation", out=sq_, in_=sq_, func=AF.Silu, reads=[sqb_], writes=[sqb_])
            for d in range(2):
                rm, rmb = rms_[d]
                mk, mkb = m32[d]
                S.dma("sp", a1, self.PS[segs[d]][rows, :], writes=[a1b])
                S.op("act", "activation", out=a1, in_=a1, func=AF.Sigmoid, reads=[a1b], writes=[a1b])
                S.op("dve", "tensor_scalar", out=a1, in0=a1, scalar1=lbt[:, 2, h:h + 1], scalar2=lbt[:, 1, h:h + 1], op0=ALU.mult, op1=ALU.add,
                     reads=[a1b, lbb], writes=[a1b])
                S.op("dve", "tensor_scalar", out=a2, in0=a1, scalar1=-1.0, scalar2=1.0, op0=ALU.mult, op1=ALU.add,
                     reads=[a1b], writes=[a2b])
                S.op("dve", "tensor_scalar", out=a1, in0=a1, scalar1=1e-30, scalar2=None, op0=ALU.max, reads=[a1b], writes=[a1b])
                S.op("act", "activation", out=a1, in_=a1, func=AF.Ln, reads=[a1b], writes=[a1b])
                if d == 0:
                    S.op("dve", "tensor_tensor_scan", out=a3, data0=rm, data1=a1, initial=0.0, op0=ALU.mult, op1=ALU.add,
                         reads=[rmb, a1b], writes=[a3b])
                else:
                    S.op("dve", "tensor_tensor_scan", out=a3[:, ::-1], data0=rm[:, ::-1], data1=a1[:, ::-1], initial=0.0,
                         op0=ALU.mult, op1=ALU.add, reads=[rmb, a1b], writes=[a3b])
                S.op("act", "activation", out=a1, in_=a3, func=AF.Exp, reads=[a3b], writes=[a1b])
                S.op("act", "activation", out=a3, in_=a3, func=AF.Exp, scale=-1.0, reads=[a3b], writes=[a3b])
                S.op("dve", "tensor_tensor", out=qt, in0=sq_, in1=a1, op=ALU.mult, reads=[sqb_, a1b], writes=[qtb])
                S.op("dve", "tensor_tensor", out=kt, in0=a2, in1=a3, op=ALU.mult, reads=[a2b, a3b], writes=[ktb])
                order = list(range(NTILE)) if d == 0 else [1, 0] + list(range(NTILE - 1, 1, -1))
                subs = (0, 1, 2, 3) if d == 0 else (3, 2, 1, 0)
                first = True
                aprev = None
                ti = 0
                for t in order:
                    tc = slice(t * 128, (t + 1) * 128)
                    ktk, ktkb = ktoks[ki % 2]
                    sm, smb = sms[ki % 2]
                    ki += 1
                    ps, pb = self.psum("s")
                    psb16 = ps[:, :].bitcast(BF16)
                    S.op("pe", "transpose", out=psb16[:, 0:128], in_=kt[:, tc], identity=self.identb, reads=[ktb, self.identb_b], writes=[pb])
                    S.op("act", "activation", out=ktk, in_=psb16[:, 0:128], func=AF.Copy, reads=[pb], writes=[ktkb])
                    stp, stpb = self.psum("s")
                    S.op("pe", "matmul", stp[:, 0:128], lhsT=kt[:, tc], rhs=qt[:, tc], start=True, stop=True, reads=[ktb, qtb], writes=[stpb])
                    S.op("dve", "tensor_tensor", out=sm, in0=stp[:, 0:128], in1=mk, op=ALU.mult, reads=[stpb, mkb], writes=[smb])
                    up, upb = self.psum("s")
                    for n in range(4):
                        S.op("pe", "matmul", up[:, n * 128:(n + 1) * 128], lhsT=ktk, rhs=vm[:, t, n, :], start=True, stop=True,
                             reads=[ktkb, vmb], writes=[upb], signal=(n == 3))
                    op_, opb = self.psum("acc")
                    n_inter = 4 - (1 if first else 0)
                    S.op("pe", "matmul", op_[:, 0:128], lhsT=V[:, t, :], rhs=sm, start=True, stop=False, reads=[Vb, smb], writes=[opb],
                         signal=False)
                    done = 0
                    for n in subs:
                        col = t * 128 + n * 32
                        if not first:
                            Sb, Sbb = Sbs[(ti + 1) % 2]
                            done += 1
                            S.op("pe", "matmul", op_[:, n * 32:(n + 1) * 32], lhsT=Sb, rhs=qt[:, col:col + 32], start=False,
                                 stop=(done == n_inter), reads=[Sbb, qtb], writes=[opb], signal=True)
                        Tn, Tnb = Ts[ti % 2]
                        if first:
                            S.op("dve", "tensor_copy", out=Tn, in_=up[:, n * 128:(n + 1) * 128], reads=[upb], writes=[Tnb])
                        else:
                            To, Tob = Ts[(ti + 1) % 2]
                            S.op("dve", "scalar_tensor_tensor", out=Tn, in0=To, scalar=aprev, in1=up[:, n * 128:(n + 1) * 128],
                                 op0=ALU.mult, op1=ALU.add, reads=[Tob, a1b, upb], writes=[Tnb])
                        ac = col + (31 if d == 0 else 0)
                        aprev = a1[:, ac:ac + 1]
                        Sbn, Sbnb = Sbs[ti % 2]
                        S.op("act", "activation", out=Sbn, in_=Tn, func=AF.Copy, scale=aprev, reads=[Tnb, a1b], writes=[Sbnb])
                        ti += 1
                        first = False
                    if d == 0:
                        S.op("dve", "tensor_copy", out=OT[:, tc], in_=op_[:, 0:128], reads=[opb], writes=[OTb])
                    else:
                        S.op("dve", "tensor_tensor", out=OT[:, tc], in0=op_[:, 0:128], in1=OT[:, tc], op=ALU.add, reads=[opb, OTb], writes=[OTb])
            S.op("act", "activation", out=sqb2, in_=OT, func=AF.Square, reads=[OTb], writes=[sqb2b])
            for (t0, nt) in chunks:
                ps, pb = self.psum()
                S.op("pe", "matmul", ps[:, 0:nt], lhsT=self.onesb, rhs=sqb2[:, t0:t0 + nt], start=True, stop=True,
                     reads=[self.onesb_b, sqb2b], writes=[pb])
                S.op("act", "activation", out=a2[:, t0:t0 + nt], in_=ps[:, 0:nt], func=AF.Sqrt, bias=self.eps_t, scale=1.0 / HD,
                     reads=[pb, self.eps_b], writes=[a2b])
            S.op("dve", "reciprocal", out=a2, in_=a2, reads=[a2b], writes=[a2b])
            S.op("dve", "scalar_tensor_tensor", out=OT, in0=OT, scalar=nw[:, h:h + 1], in1=a2, op0=ALU.mult, op1=ALU.mult,
                 reads=[OTb, nwb, a2b], writes=[OTb])
            S.dma("sp", a3, self.PS["AG"][rows, :], writes=[a3b])
            S.op("act", "activation", out=a3, in_=a3, func=AF.Silu, reads=[a3b], writes=[a3b])
            S.op("dve", "tensor_tensor", out=ob, in0=OT, in1=a3, op=ALU.mult, reads=[OTb, a3b], writes=[obb])
            S.dma("sp", self.MIXT[h * 128:(h + 1) * 128, :], ob, reads=[obb])
        A.pop()
        S.barrier()


_PROG_CACHE = {}


def _get_prog():
    if "p" not in _PROG_CACHE:
        P = MixProg(layers=(0, 1, 2, 3))
        P.build()
        _PROG_CACHE["p"] = P
    return _PROG_CACHE["p"]


def kernel(**inputs):
    inp = {k: np.asarray(v) for k, v in inputs.items()}
    P = _get_prog()
    f32 = np.float32
    shared = mixer_inputs_np(inp)
    shared.update(
        mod_down=inp["mod_down"], mod_up=inp["mod_up"], mod_b=inp["mod_b"],
        norm1_w=inp["norm1_w"].reshape(DEPTH, 32, 128), norm2_w=inp["norm2_w"].reshape(DEPTH, 32, 128),
        w_in=inp["w_in"], w_out=inp["w_out"],
        ffn_w1=inp["ffn_w1"], ffn_w3=inp["ffn_w3"], ffn_w2=inp["ffn_w2"],
        moe_router=inp["moe_router"], moe_w1=inp["moe_w1"], moe_w3=inp["moe_w3"],
        moe_w2=inp["moe_w2"].reshape(2, N_EXP * D_EXP, D), final_norm_w=inp["final_norm_w"],
    )
    B = inp["x"].shape[0]
    in_maps = []
    for b in range(B):
        m = {}
        for name in P.in_names:
            if name == "xin":
                m[name] = np.ascontiguousarray(np.concatenate([inp["ctx"][b], inp["x"][b]], axis=0).astype(f32, copy=False))
            elif name == "cvec":
                m[name] = np.ascontiguousarray(np.concatenate([inp["c"][b].reshape(32, 128), inp["c_ctx"].reshape(32, 128)], axis=0))
            else:
                m[name] = np.ascontiguousarray(shared[name])
        in_maps.append(m)
    res = run_bass_kernel_spmd(P.nc, in_maps, core_ids=list(range(B)))
    return np.stack([np.asarray(r["out"], dtype=f32) for r in res.results], axis=0)
```
